# Optimizing a Trainium2 kernel written in Bass

```python
import jax, jax.numpy as jnp
from jax import lax
import numpy as np


D_MODEL = 2048
BATCH = 4
SEQ = 4096
DEPTH = 1

GRID_W = 64
PLE_DIM = 256
NA_HEADS = 8
NA_HEAD_DIM = 128
NA_WIN_H = 8
NA_WIN_W = 16
NA_WIDTH = NA_HEADS * NA_HEAD_DIM
MLA_HEADS = 8
MLA_NOPE_DIM = 128
MLA_ROPE_DIM = 64
MLA_V_DIM = 128
MLA_QK_DIM = MLA_NOPE_DIM + MLA_ROPE_DIM
Q_LORA_RANK = 512
KV_LORA_RANK = 512
MLA_WIDTH = MLA_HEADS * MLA_V_DIM
ROPE_THETA = 10000.0
Q_BLOCK = 128
MIX_WIDTH = NA_WIDTH + MLA_WIDTH
IN_WIDTH = 3 * NA_WIDTH + Q_LORA_RANK + KV_LORA_RANK + MLA_ROPE_DIM
N_GROUPS = 8
EXPERTS_PER_GROUP = 8
N_EXPERTS = N_GROUPS * EXPERTS_PER_GROUP
TOP_K_EXPERT = 2
D_EXPERT = 512
MOE_BLOCK = 256
ALPHA = (2 * DEPTH) ** 0.25
BETA = (8 * DEPTH) ** -0.25
LN_EPS = 1e-5
RMS_EPS = 1e-6

kernel_name = 'hybrid_na_mla_hmoe_block'


def layer_norm(x, g, b):
    xf = x.astype(jnp.float32)
    mu = jnp.mean(xf, axis=-1, keepdims=True)
    var = jnp.mean(jnp.square(xf - mu), axis=-1, keepdims=True)
    y = (xf - mu) * lax.rsqrt(var + LN_EPS)
    return (y * g.astype(jnp.float32) + b.astype(jnp.float32)).astype(x.dtype)


def rms_norm(x, g):
    xf = x.astype(jnp.float32)
    y = xf * lax.rsqrt(jnp.mean(jnp.square(xf), axis=-1, keepdims=True) + RMS_EPS)
    return (y * g.astype(jnp.float32)).astype(x.dtype)


def rope_tables(positions, dtype):
    inv_freq = 1.0 / (ROPE_THETA ** (jnp.arange(0, MLA_ROPE_DIM, 2, dtype=jnp.float32) / MLA_ROPE_DIM))
    ang = positions.astype(jnp.float32)[..., None] * inv_freq
    return jnp.cos(ang).astype(dtype), jnp.sin(ang).astype(dtype)


def apply_rope(x, cos, sin):
    x1, x2 = jnp.split(x, 2, axis=-1)
    return jnp.concatenate([x1 * cos - x2 * sin, x1 * sin + x2 * cos], axis=-1)


def neighbourhood_attention(q, k, v, rpb):
    B, S, H, Dh = q.shape
    rows = S // GRID_W
    kh = min(NA_WIN_H, rows)
    kw = NA_WIN_W
    scale = Dh ** -0.5
    qg = q.reshape(B, rows, GRID_W, H, Dh)
    kg = k.reshape(B, rows, GRID_W, H, Dh)
    vg = v.reshape(B, rows, GRID_W, H, Dh)
    col = jnp.arange(GRID_W)
    c0 = jnp.clip(col - kw // 2, 0, GRID_W - kw)
    col_idx = c0[:, None] + jnp.arange(kw)[None, :]
    col_off = col_idx - col[:, None] + (NA_WIN_W - 1)
    rpb_c = rpb[:, :, col_off]
    r0 = jnp.clip(jnp.arange(rows) - kh // 2, 0, rows - kh)

    def one_row(r):
        start = r0[r]
        q_r = lax.dynamic_index_in_dim(qg, r, axis=1, keepdims=False)
        k_rows = lax.dynamic_slice_in_dim(kg, start, kh, axis=1)
        v_rows = lax.dynamic_slice_in_dim(vg, start, kh, axis=1)
        k_win = k_rows[:, :, col_idx]
        v_win = v_rows[:, :, col_idx]
        row_off = start + jnp.arange(kh) - r + (NA_WIN_H - 1)
        bias = rpb_c[:, row_off].transpose(0, 2, 1, 3)
        s = jnp.einsum('bqhd,brqwhd->bhqrw', q_r, k_win).astype(jnp.float32) * scale
        s = s + bias.astype(jnp.float32)[None]
        pr = jax.nn.softmax(s.reshape(B, H, GRID_W, kh * kw), axis=-1)
        pr = pr.reshape(s.shape).astype(v.dtype)
        return jnp.einsum('bhqrw,brqwhd->bqhd', pr, v_win)

    out = lax.map(one_row, jnp.arange(rows))
    return out.transpose(1, 0, 2, 3, 4).reshape(B, S, H * Dh)


def dense_attention_blocked(q, k, v, scale):
    B, S, H, Dq = q.shape
    Dv = v.shape[-1]
    qb = q.reshape(B, S // Q_BLOCK, Q_BLOCK, H, Dq).transpose(1, 0, 2, 3, 4)

    def block(qi):
        s = jnp.einsum('bqhd,bkhd->bhqk', qi, k).astype(jnp.float32) * scale
        pr = jax.nn.softmax(s, axis=-1).astype(v.dtype)
        return jnp.einsum('bhqk,bkhd->bqhd', pr, v)

    o = lax.map(block, qb)
    return o.transpose(1, 0, 2, 3, 4).reshape(B, S, H * Dv)


def token_mixer(h, cos, sin, w_in, rpb, q_norm_g, kv_norm_g, w_uq, w_uk, w_uv, w_o):
    B, S, _ = h.shape
    proj = jnp.einsum('bsd,df->bsf', h, w_in)
    s1 = NA_WIDTH
    s2 = 2 * NA_WIDTH
    s3 = 3 * NA_WIDTH
    s4 = s3 + Q_LORA_RANK
    s5 = s4 + KV_LORA_RANK
    q_na = proj[..., :s1].reshape(B, S, NA_HEADS, NA_HEAD_DIM)
    k_na = proj[..., s1:s2].reshape(B, S, NA_HEADS, NA_HEAD_DIM)
    v_na = proj[..., s2:s3].reshape(B, S, NA_HEADS, NA_HEAD_DIM)
    c_q = proj[..., s3:s4]
    c_kv = proj[..., s4:s5]
    k_r = proj[..., s5:]
    o_na = neighbourhood_attention(q_na, k_na, v_na, rpb)
    q = jnp.einsum('bsr,rf->bsf', rms_norm(c_q, q_norm_g), w_uq).reshape(B, S, MLA_HEADS, MLA_QK_DIM)
    q_nope, q_pe = q[..., :MLA_NOPE_DIM], q[..., MLA_NOPE_DIM:]
    q_pe = apply_rope(q_pe, cos[:, :, None, :], sin[:, :, None, :])
    c_kv = rms_norm(c_kv, kv_norm_g)
    k_nope = jnp.einsum('bsr,rf->bsf', c_kv, w_uk).reshape(B, S, MLA_HEADS, MLA_NOPE_DIM)
    v = jnp.einsum('bsr,rf->bsf', c_kv, w_uv).reshape(B, S, MLA_HEADS, MLA_V_DIM)
    k_pe = apply_rope(k_r, cos, sin)
    k_pe = jnp.broadcast_to(k_pe[:, :, None, :], (B, S, MLA_HEADS, MLA_ROPE_DIM))
    q_full = jnp.concatenate([q_nope, q_pe], axis=-1)
    k_full = jnp.concatenate([k_nope, k_pe], axis=-1)
    o_mla = dense_attention_blocked(q_full, k_full, v, MLA_QK_DIM ** -0.5)
    o = jnp.concatenate([o_na, o_mla], axis=-1)
    return jnp.einsum('bsf,fd->bsd', o, w_o)


def hier_moe(h, w_group, b_group, w_router, b_router, w_gate, w_up, w_down):
    B, S, D = h.shape
    N = B * S
    t = h.reshape(N, D)
    g_prob = jax.nn.softmax(jnp.einsum('nd,dg->ng', t, w_group).astype(jnp.float32) + b_group.astype(jnp.float32), axis=-1)
    g_val, g_idx = lax.top_k(g_prob, 1)
    e_logits = jnp.einsum('nd,de->ne', t, w_router).astype(jnp.float32).reshape(N, N_GROUPS, EXPERTS_PER_GROUP)
    e_logits = e_logits + b_router.astype(jnp.float32)
    e_sel = jnp.take_along_axis(e_logits, g_idx[:, :, None], axis=1)[:, 0]
    e_prob = jax.nn.softmax(e_sel, axis=-1)
    e_val, e_idx = lax.top_k(e_prob, TOP_K_EXPERT)
    e_val = e_val / jnp.sum(e_val, axis=-1, keepdims=True)
    gate = g_val * e_val
    eid = g_idx * EXPERTS_PER_GROUP + e_idx
    M = N * TOP_K_EXPERT
    flat_eid = eid.reshape(-1)
    flat_tok = jnp.repeat(jnp.arange(N), TOP_K_EXPERT)
    flat_w = gate.reshape(-1)
    order = jnp.argsort(flat_eid)
    s_eid = flat_eid[order]
    s_tok = flat_tok[order]
    s_w = flat_w[order]
    counts = jnp.bincount(flat_eid, length=N_EXPERTS)
    starts = jnp.cumsum(counts) - counts
    padded = ((counts + MOE_BLOCK - 1) // MOE_BLOCK) * MOE_BLOCK
    pad_ends = jnp.cumsum(padded)
    pad_starts = pad_ends - padded
    pos = pad_starts[s_eid] + (jnp.arange(M) - starts[s_eid])
    n_blocks = (M + N_EXPERTS * (MOE_BLOCK - 1) + MOE_BLOCK - 1) // MOE_BLOCK
    P = n_blocks * MOE_BLOCK
    xs = jnp.zeros((P, D), t.dtype).at[pos].set(t[s_tok])
    block_e = jnp.clip(jnp.searchsorted(pad_ends, jnp.arange(n_blocks) * MOE_BLOCK, side='right'), 0, N_EXPERTS - 1)

    def expert_block(args):
        xb, e = args
        hdn = jax.nn.silu(xb @ w_gate[e]) * (xb @ w_up[e])
        return hdn @ w_down[e]

    ys = lax.map(expert_block, (xs.reshape(n_blocks, MOE_BLOCK, D), block_e)).reshape(P, D)
    y = jnp.zeros((N, D), t.dtype).at[s_tok].add(ys[pos] * s_w[:, None].astype(t.dtype))
    return y.reshape(B, S, D)


def per_layer_embedding(h, p_i, w_ple, w_ple_gate, ple_norm_g):
    gate = jax.nn.sigmoid(jnp.einsum('bsd,de->bse', h, w_ple_gate).astype(jnp.float32))
    e = jnp.einsum('bsq,qd->bsd', p_i, w_ple)
    return rms_norm(e * gate.astype(e.dtype), ple_norm_g)


def setup_inputs(seed: int = 0) -> dict:
    key = jax.random.key(seed)
    ks = jax.random.split(key, 24)

    def nrm(k, shape, scale):
        return jax.random.normal(k, shape, jnp.float32) * scale

    D = D_MODEL
    x = nrm(ks[0], (BATCH, SEQ, D), 1.0)
    p = nrm(ks[1], (DEPTH, BATCH, SEQ, PLE_DIM), 1.0)
    positions = jnp.tile(jnp.arange(SEQ, dtype=jnp.int32)[None, :], (BATCH, 1))
    col_scale = jnp.ones((IN_WIDTH,), jnp.float32).at[2 * NA_WIDTH:3 * NA_WIDTH].set(BETA)
    w_in = nrm(ks[2], (DEPTH, D, IN_WIDTH), D ** -0.5) * col_scale
    rpb = nrm(ks[3], (DEPTH, NA_HEADS, 2 * NA_WIN_H - 1, 2 * NA_WIN_W - 1), 0.02)
    q_norm_g = 1.0 + nrm(ks[4], (DEPTH, Q_LORA_RANK), 0.02)
    kv_norm_g = 1.0 + nrm(ks[5], (DEPTH, KV_LORA_RANK), 0.02)
    w_uq = nrm(ks[6], (DEPTH, Q_LORA_RANK, MLA_HEADS * MLA_QK_DIM), Q_LORA_RANK ** -0.5)
    w_uk = nrm(ks[7], (DEPTH, KV_LORA_RANK, MLA_HEADS * MLA_NOPE_DIM), KV_LORA_RANK ** -0.5)
    w_uv = nrm(ks[8], (DEPTH, KV_LORA_RANK, MLA_HEADS * MLA_V_DIM), KV_LORA_RANK ** -0.5 * BETA)
    w_o = nrm(ks[9], (DEPTH, MIX_WIDTH, D), MIX_WIDTH ** -0.5 * BETA)
    ln1_g = 1.0 + nrm(ks[10], (DEPTH, D), 0.02)
    ln1_b = nrm(ks[11], (DEPTH, D), 0.02)
    w_group = nrm(ks[12], (DEPTH, D, N_GROUPS), D ** -0.5)
    b_group = nrm(ks[13], (DEPTH, N_GROUPS), 0.01)
    w_router = nrm(ks[14], (DEPTH, D, N_EXPERTS), D ** -0.5)
    b_router = nrm(ks[15], (DEPTH, N_GROUPS, EXPERTS_PER_GROUP), 0.01)
    w_gate = nrm(ks[16], (DEPTH, N_EXPERTS, D, D_EXPERT), D ** -0.5)
    w_up = nrm(ks[17], (DEPTH, N_EXPERTS, D, D_EXPERT), D ** -0.5 * BETA)
    w_down = nrm(ks[18], (DEPTH, N_EXPERTS, D_EXPERT, D), D_EXPERT ** -0.5 * BETA)
    ln2_g = 1.0 + nrm(ks[19], (DEPTH, D), 0.02)
    ln2_b = nrm(ks[20], (DEPTH, D), 0.02)
    w_ple = nrm(ks[21], (DEPTH, PLE_DIM, D), PLE_DIM ** -0.5)
    w_ple_gate = nrm(ks[22], (DEPTH, D, D), D ** -0.5)
    ple_norm_g = 1.0 + nrm(ks[23], (DEPTH, D), 0.02)
    return {'x': x, 'p': p, 'positions': positions, 'w_in': w_in, 'rpb': rpb,
            'q_norm_g': q_norm_g, 'kv_norm_g': kv_norm_g, 'w_uq': w_uq, 'w_uk': w_uk,
            'w_uv': w_uv, 'w_o': w_o, 'ln1_g': ln1_g, 'ln1_b': ln1_b,
            'w_group': w_group, 'b_group': b_group, 'w_router': w_router, 'b_router': b_router,
            'w_gate': w_gate, 'w_up': w_up, 'w_down': w_down, 'ln2_g': ln2_g, 'ln2_b': ln2_b,
            'w_ple': w_ple, 'w_ple_gate': w_ple_gate, 'ple_norm_g': ple_norm_g}


def reference(x, p, positions, w_in, rpb, q_norm_g, kv_norm_g, w_uq, w_uk, w_uv, w_o,
              ln1_g, ln1_b, w_group, b_group, w_router, b_router, w_gate, w_up, w_down,
              ln2_g, ln2_b, w_ple, w_ple_gate, ple_norm_g):
    cos, sin = rope_tables(positions, x.dtype)
    for i in range(DEPTH):
        mix = token_mixer(x, cos, sin, w_in[i], rpb[i], q_norm_g[i], kv_norm_g[i],
                          w_uq[i], w_uk[i], w_uv[i], w_o[i])
        x = layer_norm(ALPHA * x + mix, ln1_g[i], ln1_b[i])
        ffn = hier_moe(x, w_group[i], b_group[i], w_router[i], b_router[i],
                       w_gate[i], w_up[i], w_down[i])
        x = layer_norm(ALPHA * x + ffn, ln2_g[i], ln2_b[i])
        x = x + per_layer_embedding(x, p[i], w_ple[i], w_ple_gate[i], ple_norm_g[i])
    return x
```

```python
import math
import os
import numpy as np
from contextlib import ExitStack
import concourse.bass as bass
import concourse.mybir as mybir
from concourse.bass_utils import run_bass_kernel_spmd

F32 = mybir.dt.float32
BF16 = mybir.dt.bfloat16
I32 = mybir.dt.int32
U8 = mybir.dt.uint8
AF = mybir.ActivationFunctionType
ALU = mybir.AluOpType
AX = mybir.AxisListType
DTSIZE = {F32: 4, BF16: 2, I32: 4, U8: 1}

D = 2048
NTOK = 2048
ALPHA = 2.0 ** 0.25
LN_EPS = 1e-5
RMS_EPS = 1e-6
NSLOT = 8192
NROWS = NSLOT + 128
PRESTAGE = False
NPRE = 32
SBUF_CAP = 200 * 1024
PI = math.pi


class SemRec:
    def __init__(self, h):
        self.h = h
        self.count = 0


class Res:
    __slots__ = ("w", "r_eng", "r_dma", "dsem")

    def __init__(self):
        self.w = None
        self.r_eng = {}
        self.r_dma = {}
        self.dsem = None


class Eng:
    def __init__(self, name, h, sem):
        self.name = name
        self.h = h
        self.sem = sem
        self.count = 0
        self.known = {}
        self.knownd = {}


class T:
    def __init__(self, ap):
        self.ap = ap
        self.res = Res()

    def v3(self, k):
        return self.ap.rearrange("p (k f) -> p k f", k=k)


class Ctx:
    def __init__(self, nc, es):
        self.nc = nc
        self.es = es
        self.engs = {}
        for name, h in (("pe", nc.tensor), ("act", nc.scalar), ("dve", nc.vector),
                        ("pool", nc.gpsimd), ("sp", nc.sync)):
            sem = SemRec(es.enter_context(nc.semaphore("e_" + name)))
            self.engs[name] = Eng(name, h, sem)
        self.dsems = []
        self.n_inst = 0

    def new_dsem(self, name):
        s = SemRec(self.es.enter_context(self.nc.semaphore(name)))
        self.dsems.append(s)
        return s

    def _wait(self, eng, tok):
        if tok[0] == "e":
            src, c = tok[1], tok[2]
            if src is eng and eng.name == "pe":
                return
            if eng.known.get(src.name, 0) >= c:
                return
            eng.h.wait_ge(src.sem.h, c)
            eng.known[src.name] = c
        else:
            s = tok[1]
            v = s.count
            if eng.knownd.get(id(s), 0) >= v:
                return
            eng.h.wait_ge(s.h, v)
            eng.knownd[id(s)] = v

    def _deps(self, eng, reads, writes):
        for r in reads:
            if r.w is not None:
                self._wait(eng, r.w)
        for w in writes:
            if w.w is not None:
                self._wait(eng, w.w)
            for en, c in w.r_eng.items():
                self._wait(eng, ("e", self.engs[en], c))
            for s in w.r_dma.values():
                self._wait(eng, ("d", s))

    def op(self, ename, fn, reads=(), writes=()):
        eng = self.engs[ename]
        self._deps(eng, reads, writes)
        inst = fn()
        eng.count += 1
        inst.then_inc(eng.sem.h, 1)
        eng.known[eng.name] = max(eng.known.get(eng.name, 0), 0)
        tok = ("e", eng, eng.count)
        for r in reads:
            r.r_eng[eng.name] = eng.count
        for w in writes:
            w.w = tok
            w.r_eng = {}
            w.r_dma = {}
        self.n_inst += 1

    def dma(self, q, fn, semres, reads=(), writes=()):
        eng = self.engs[q]
        self._deps(eng, reads, writes)
        if semres.dsem is None:
            semres.dsem = self.new_dsem("d%d" % len(self.dsems))
        s = semres.dsem
        inst = fn()
        s.count += 16
        inst.then_inc(s.h, 16)
        tok = ("d", s)
        for r in reads:
            r.r_dma[id(s)] = s
        for w in writes:
            w.w = tok
            w.r_eng = {}
            w.r_dma = {}
        self.n_inst += 1

    def barrier(self, exclude=()):
        for e in self.engs.values():
            for o in self.engs.values():
                if o is not e and o.count > 0:
                    self._wait(e, ("e", o, o.count))
            for s in self.dsems:
                if s.count > 0 and s not in exclude:
                    self._wait(e, ("d", s))


class SB:
    def __init__(self, big, cap):
        self.big = big
        self.cap = cap
        self.off = 0

    def alloc(self, cols, dtype, parts=128):
        size = cols * DTSIZE[dtype]
        size = (size + 63) // 64 * 64
        assert self.off + size <= self.cap, ("SBUF overflow", self.off, size, self.cap)
        ap = self.big[0:parts, self.off:self.off + size]
        if dtype != U8:
            ap = ap.bitcast(dtype)
        ap = ap[:, 0:cols]
        self.off += size
        return T(ap)


def build_program(debug=False):
    nc = bass.Bass("TRN2", target_bir_lowering=False)
    es = ExitStack()

    def din(name, shape, dt=F32):
        return nc.dram_tensor(name, list(shape), dt, kind="ExternalInput").ap()

    xT_own = din("xT_own", [D, NTOK])
    xT_oth = din("xT_oth", [D, NTOK])
    xT_halo = din("xT_halo", [D, 768])
    x_own = din("x_own", [NTOK, D])
    pT_d = din("pT", [256, NTOK])
    posb = din("posb", [64, 4096], I32)
    w_in = din("w_in", [D, 4160])
    w_krsw = din("w_krsw", [D, 64])
    w_uq = din("w_uq", [512, 1536])
    w_uqsw = din("w_uqsw", [512, 512])
    w_uk = din("w_uk", [512, 1024])
    w_uv = din("w_uv", [512, 1024])
    qg_d = din("qg", [128, 4])
    kvg_d = din("kvg", [128, 4])
    w_o = din("w_o", [D, D])
    ln1g_d = din("ln1g", [128, D])
    ln1b_d = din("ln1b", [128, D])
    ln2g_d = din("ln2g", [128, D])
    ln2b_d = din("ln2b", [128, D])
    pleg_d = din("pleg", [128, D])
    w_rt = din("w_rt", [D, 72])
    b_rt = din("b_rt", [128, 72])
    w_gate = din("w_gate", [64, D, 512])
    w_up = din("w_up", [64, D, 512])
    w_down = din("w_down", [64, 512, D])
    w_ple = din("w_ple", [256, D])
    w_pg = din("w_pg", [D, D])
    natab = din("natab", [8, 5, 128, 896])
    ident_d = din("ident", [128, 128])
    tri_d = din("tri", [128, 128])
    ropec_d = din("ropec", [64, 4])
    ebase_d = din("ebase", [128, 64])
    dummy_d = din("dummyc", [128, 1])

    out_d = nc.dram_tensor("out", [NTOK, D], F32, kind="ExternalOutput").ap()
    skind = "ExternalOutput" if debug else "Internal"
    oT_d = nc.dram_tensor("oT_s", [16, 128, NTOK], BF16, kind=skind).ap()
    x1s_d = nc.dram_tensor("x1_s", [NTOK, D], F32, kind=skind).ap()
    xs_d = nc.dram_tensor("xs_s", [NROWS, D], BF16, kind="Internal").ap()
    ys_d = nc.dram_tensor("ys_s", [NROWS, D], F32, kind="Internal").ap()
    rt_d = nc.dram_tensor("rt_s", [128, 64], F32, kind=skind).ap()
    if PRESTAGE:
        wgb_d = nc.dram_tensor("wgb_s", [NPRE, 128, 16 * 512], BF16, kind="Internal").ap()
        wub_d = nc.dram_tensor("wub_s", [NPRE, 128, 16 * 512], BF16, kind="Internal").ap()
        wdb_d = nc.dram_tensor("wdb_s", [NPRE, 128, 4 * D], BF16, kind="Internal").ap()

    big = es.enter_context(nc.sbuf_tensor("big", [128, SBUF_CAP], U8))
    ps = es.enter_context(nc.psum_tensor("ps", [128, 8, 512], F32))
    C = Ctx(nc, es)
    sb = SB(big, SBUF_CAP)
    pres = [Res() for _ in range(8)]
    BR = nc.gpsimd.to_reg(NROWS - 1)

    def psb(b, c0=0, c1=512, parts=128):
        return ps[0:parts, b, c0:c1]

    def psbf(b, c0, c1):
        return ps[:, b, :].bitcast(BF16)[:, c0:c1]

    def mm(out, lhsT, rhs, start, stop, reads, writes):
        C.op("pe", lambda: nc.tensor.matmul(out, lhsT, rhs, start=start, stop=stop), reads, writes)

    def tr(out, in_, ident, reads, writes):
        C.op("pe", lambda: nc.tensor.transpose(out, in_, ident), reads, writes)

    def act(out, in_, func, reads, writes, **kw):
        C.op("act", lambda: nc.scalar.activation(out=out, in_=in_, func=func, **kw), reads, writes)

    def tt(out, in0, in1, op, reads, writes):
        C.op("dve", lambda: nc.vector.tensor_tensor(out=out, in0=in0, in1=in1, op=op), reads, writes)

    def ts(out, in0, s1, s2, op0, op1, reads, writes):
        if s2 is None:
            C.op("dve", lambda: nc.vector.tensor_scalar(out=out, in0=in0, scalar1=s1, scalar2=None, op0=op0),
                 reads, writes)
        else:
            C.op("dve", lambda: nc.vector.tensor_scalar(out=out, in0=in0, scalar1=s1, scalar2=s2, op0=op0, op1=op1),
                 reads, writes)

    def stt(out, in0, scalar, in1, op0, op1, reads, writes):
        C.op("dve", lambda: nc.vector.scalar_tensor_tensor(out=out, in0=in0, scalar=scalar, in1=in1, op0=op0, op1=op1),
             reads, writes)

    def vcopy(out, in_, reads, writes):
        C.op("dve", lambda: nc.vector.tensor_copy(out=out, in_=in_), reads, writes)

    def recip(out, in_, reads, writes):
        C.op("dve", lambda: nc.vector.reciprocal(out=out, in_=in_), reads, writes)

    def rmax(out, in_, reads, writes):
        C.op("dve", lambda: nc.vector.reduce_max(out=out, in_=in_, axis=AX.X), reads, writes)

    def rsum(out, in_, reads, writes):
        C.op("dve", lambda: nc.vector.reduce_sum(out=out, in_=in_, axis=AX.X), reads, writes)

    def ld(q, t_ap, d_ap, semres, writes):
        C.dma(q, lambda: C.engs[q].h.dma_start(out=t_ap, in_=d_ap), semres, (), writes)

    def st(q, d_ap, t_ap, semres, reads):
        C.dma(q, lambda: C.engs[q].h.dma_start(out=d_ap, in_=t_ap), semres, reads, ())

    def kp(dram2d, p=128):
        return dram2d.rearrange("(k p) f -> p k f", p=p)

    ident_f = sb.alloc(128, F32)
    ident_b = sb.alloc(128, BF16)
    ones_b = sb.alloc(128, BF16)
    ones_f = sb.alloc(128, F32)
    tri_b = sb.alloc(128, BF16)
    ropec = sb.alloc(4, F32, parts=64)
    ebase = sb.alloc(64, F32)
    dummyc = sb.alloc(1, F32)
    slot1_i = sb.alloc(16, I32)
    slot2_i = sb.alloc(16, I32)
    g1s = sb.alloc(16, F32)
    g2s = sb.alloc(16, F32)
    cum = sb.alloc(64, F32)
    epsc = sb.alloc(4, F32)
    ld("sp", ident_f.ap, ident_d, ident_f.res, [ident_f.res])
    ld("pool", ident_b.ap, ident_d, ident_b.res, [ident_b.res])
    ld("pool", tri_b.ap, tri_d, tri_b.res, [tri_b.res])
    ld("sp", ropec.ap, ropec_d, ropec.res, [ropec.res])
    ld("sp", ebase.ap, ebase_d, ebase.res, [ebase.res])
    ld("sp", dummyc.ap, dummy_d, dummyc.res, [dummyc.res])
    C.op("dve", lambda: nc.vector.memset(ones_b.ap, 1.0), (), [ones_b.res])
    C.op("dve", lambda: nc.vector.memset(ones_f.ap, 1.0), (), [ones_f.res])
    C.op("dve", lambda: nc.vector.memset(cum.ap, 0.0), (), [cum.res])
    C.op("dve", lambda: nc.vector.memset(epsc.ap[:, 0:1], LN_EPS), (), [epsc.res])
    C.op("dve", lambda: nc.vector.memset(epsc.ap[:, 1:2], RMS_EPS), (), [epsc.res])
    C.op("dve", lambda: nc.vector.memset(epsc.ap[:, 2:3], RMS_EPS * 192.0), (), [epsc.res])
    C.op("dve", lambda: nc.vector.memset(epsc.ap[:, 3:4], 0.0), (), [epsc.res])
    base_off = sb.off

    pst = Res()
    pst_items = []
    for e_ in range(NPRE if PRESTAGE else 0):
        pst_items.append((wgb_d[e_].rearrange("p (k f) -> p k f", k=16), w_gate[e_].rearrange("(k p) f -> p k f", p=128)))
        pst_items.append((wub_d[e_].rearrange("p (k f) -> p k f", k=16), w_up[e_].rearrange("(k p) f -> p k f", p=128)))
        pst_items.append((wdb_d[e_].rearrange("p (k f) -> p k f", k=4), w_down[e_].rearrange("(k p) f -> p k f", p=128)))
    pst_i = [0]

    def prestage(n):
        if not PRESTAGE:
            return
        for _ in range(n):
            if pst_i[0] >= len(pst_items):
                return
            o_, i_ = pst_items[pst_i[0]]
            pst_i[0] += 1
            C.dma("pool", lambda: nc.gpsimd.dma_start(out=o_, in_=i_), pst, (), ())

    def pbarrier():
        C.barrier(exclude=(pst.dsem,) if pst.dsem is not None else ())

    xo = sb.alloc(16 * 2048, BF16)
    xh = sb.alloc(16 * 768, BF16)
    wqs = [sb.alloc(16 * 128, BF16) for _ in range(2)]
    wks = [sb.alloc(16 * 128, BF16) for _ in range(2)]
    wvs = [sb.alloc(16 * 128, BF16) for _ in range(2)]
    qT = sb.alloc(2048, BF16)
    kT = sb.alloc(22 * 128, BF16)
    vN = sb.alloc(22 * 128, BF16)
    tabs = [sb.alloc(896, F32) for _ in range(2)]
    scs = [sb.alloc(896, F32) for _ in range(2)]
    pTs = [sb.alloc(896, BF16) for _ in range(2)]
    rdn = [sb.alloc(128, F32) for _ in range(2)]
    ost = [sb.alloc(2048, BF16) for _ in range(2)]

    xo3 = xo.v3(16)
    xh3 = xh.v3(16)
    for g in range(4):
        C.dma("pool", lambda g=g: nc.gpsimd.dma_start(out=xo3[:, 4 * g:4 * g + 4, :], in_=kp(xT_own)[:, 4 * g:4 * g + 4, :]),
              xo.res, (), [xo.res])
    C.dma("pool", lambda: nc.gpsimd.dma_start(out=xh3, in_=kp(xT_halo)), xh.res, (), [xh.res])

    def ext_src(e):
        if e < 3:
            return xh, xh3, e * 128
        if e < 19:
            return xo, xo3, (e - 3) * 128
        return xh, xh3, (e - 19 + 3) * 128

    tab_loaded = [0]
    tab_list = [(h, ty) for h in range(8) for ty in range(5)]

    def ensure_tab(li):
        while tab_loaded[0] <= min(li + 1, len(tab_list) - 1):
            i = tab_loaded[0]
            hh, ty = tab_list[i]
            t = tabs[i % 2]
            ld("sp", t.ap, natab[hh, ty], t.res, [t.res])
            tab_loaded[0] += 1

    pcnt = [0]

    def pbank():
        pcnt[0] += 1
        return 6 + (pcnt[0] % 2)

    na_scale = 128.0 ** -0.5
    ucnt = 0
    for h in range(8):
        s = h % 2
        for (wt, c0) in ((wqs[s], h * 128), (wks[s], 1024 + h * 128), (wvs[s], 2048 + h * 128)):
            C.dma("pool", lambda wt=wt, c0=c0: nc.gpsimd.dma_start(out=wt.v3(16), in_=kp(w_in[:, c0:c0 + 128])),
                  wt.res, (), [wt.res])
        wq3, wk3, wv3 = wqs[s].v3(16), wks[s].v3(16), wvs[s].v3(16)
        for tb in range(4):
            b = pbank()
            for kc in range(16):
                mm(psb(b), wq3[:, kc, :], xo3[:, kc, tb * 512:(tb + 1) * 512], kc == 0, kc == 15,
                   [wqs[s].res, xo.res], [pres[b]])
            act(qT.ap[:, tb * 512:(tb + 1) * 512], psb(b), AF.Copy, [pres[b]], [qT.res], scale=na_scale)
        kblocks = [(xh, xh3, 0, 384, 0)] + [(xo, xo3, tb * 512, 512, 384 + tb * 512) for tb in range(4)] + \
                  [(xh, xh3, 384, 384, 384 + 2048)]
        for (xt_, x3, c0, n, o0) in kblocks:
            b = pbank()
            for kc in range(16):
                mm(psb(b, 0, n), wk3[:, kc, :], x3[:, kc, c0:c0 + n], kc == 0, kc == 15,
                   [wks[s].res, xt_.res], [pres[b]])
            act(kT.ap[:, o0:o0 + n], psb(b, 0, n), AF.Copy, [pres[b]], [kT.res])
        for g0 in range(0, 22, 4):
            n = min(4, 22 - g0)
            b = pbank()
            for gi in range(n):
                xt_, x3, c0 = ext_src(g0 + gi)
                for kc in range(16):
                    mm(psb(b, gi * 128, (gi + 1) * 128), x3[:, kc, c0:c0 + 128], wv3[:, kc, :], kc == 0, kc == 15,
                       [wvs[s].res, xt_.res], [pres[b]])
            act(vN.ap[:, g0 * 128:(g0 + n) * 128], psb(b, 0, n * 128), AF.Copy, [pres[b]], [vN.res])
        o_t = ost[h % 2]

        def na_S(j, u):
            b0 = 2 * u
            for ci in range(7):
                e = j + ci
                bb, cc = (b0, ci * 128) if ci < 4 else (b0 + 1, (ci - 4) * 128)
                mm(psb(bb, cc, cc + 128), kT.ap[:, e * 128:(e + 1) * 128], qT.ap[:, j * 128:(j + 1) * 128], True, True,
                   [kT.res, qT.res], [pres[bb]])

        def na_softmax(j, u):
            ty = 0 if j == 0 else 1 if j == 1 else 2 if j < 14 else j - 11
            li = h * 5 + ty
            ensure_tab(li)
            tb_ = tabs[li % 2]
            b0 = 2 * u
            sc, pt = scs[u], pTs[u]
            tt(sc.ap[:, 0:512], psb(b0), tb_.ap[:, 0:512], ALU.add, [pres[b0], tb_.res], [sc.res])
            tt(sc.ap[:, 512:896], psb(b0 + 1, 0, 384), tb_.ap[:, 512:896], ALU.add, [pres[b0 + 1], tb_.res], [sc.res])
            act(pt.ap, sc.ap, AF.Exp, [sc.res], [pt.res])

        na_S(0, ucnt % 2)
        na_softmax(0, ucnt % 2)
        for j in range(16):
            u = ucnt % 2
            ucnt += 1
            ob = 4 + u
            if j + 1 < 16:
                na_S(j + 1, 1 - u)
                na_softmax(j + 1, 1 - u)
            pt = pTs[u]
            for ci in range(7):
                e = j + ci
                mm(psb(ob, 0, 128), vN.ap[:, e * 128:(e + 1) * 128], pt.ap[:, ci * 128:(ci + 1) * 128], ci == 0, ci == 6,
                   [vN.res, pt.res], [pres[ob]])
            for ci in range(7):
                mm(psb(ob, 256, 384), ones_b.ap, pt.ap[:, ci * 128:(ci + 1) * 128], ci == 0, ci == 6,
                   [ones_b.res, pt.res], [pres[ob]])
            rd = rdn[u]
            recip(rd.ap, psb(ob, 256, 384), [pres[ob]], [rd.res])
            tt(o_t.ap[:, j * 128:(j + 1) * 128], psb(ob, 0, 128), rd.ap, ALU.mult, [pres[ob], rd.res], [o_t.res])
        st("sp", oT_d[h], o_t.ap, o_t.res, [o_t.res])

    pbarrier()
    sb.off = base_off

    cqn = sb.alloc(4 * 2048, BF16)
    ckvn = sb.alloc(4 * 4096, BF16)
    kpe = sb.alloc(4096, BF16, parts=64)
    cos_o = sb.alloc(2048, F32, parts=64)
    sin_o = sb.alloc(2048, F32, parts=64)
    qg = sb.alloc(4, F32)
    kvg = sb.alloc(4, F32)
    ld("sp", qg.ap, qg_d, qg.res, [qg.res])
    ld("sp", kvg.ap, kvg_d, kvg.res, [kvg.res])
    cqn3 = cqn.v3(4)
    ckvn3 = ckvn.v3(4)
    b_off = sb.off

    wkv = sb.alloc(16 * 640, BF16)
    xst = [sb.alloc(16 * 512, BF16) for _ in range(2)]
    cf = sb.alloc(4 * 512, F32)
    sq = sb.alloc(4 * 512, F32)
    rs = sb.alloc(512, F32)
    rinv = sb.alloc(512, F32)
    posi = sb.alloc(512, I32, parts=64)
    ra = sb.alloc(512, F32, parts=64)
    rb = sb.alloc(512, F32, parts=64)
    rki = sb.alloc(512, I32, parts=64)
    rc_ = sb.alloc(512, F32, parts=64)
    cos_t = sb.alloc(512, F32, parts=64)
    sin_t = sb.alloc(512, F32, parts=64)
    t1 = sb.alloc(512, F32, parts=64)
    t2 = sb.alloc(512, F32, parts=64)
    wkv3 = wkv.v3(16)
    C.dma("pool", lambda: nc.gpsimd.dma_start(out=wkv3[:, :, 0:576], in_=kp(w_in[:, 3584:4160])), wkv.res, (), [wkv.res])
    C.dma("pool", lambda: nc.gpsimd.dma_start(out=wkv3[:, :, 576:640], in_=kp(w_krsw)), wkv.res, (), [wkv.res])

    def rope_tables(tok0, cos_ap, cos_res, sin_ap, sin_res):
        ld("sp", posi.ap, posb[:, tok0:tok0 + 512], posi.res, [posi.res])
        vcopy(ra.ap, posi.ap, [posi.res], [ra.res])
        ts(ra.ap, ra.ap, ropec.ap[:, 0:1], None, ALU.mult, None, [ra.res, ropec.res], [ra.res])
        for which in (0, 1):
            if which == 1:
                ts(rb.ap, ra.ap, PI / 2, None, ALU.add, None, [ra.res], [rb.res])
                src = rb
            else:
                src = ra
            ts(rc_.ap, src.ap, 1.0 / (2 * PI), None, ALU.mult, None, [src.res], [rc_.res])
            vcopy(rki.ap, rc_.ap, [rc_.res], [rki.res])
            vcopy(rc_.ap, rki.ap, [rki.res], [rc_.res])
            stt(rc_.ap, rc_.ap, -2 * PI, src.ap, ALU.mult, ALU.add, [rc_.res, src.res], [rc_.res])
            ts(t1.ap, rc_.ap, PI, -2 * PI, ALU.is_gt, ALU.mult, [rc_.res], [t1.res])
            tt(rc_.ap, rc_.ap, t1.ap, ALU.add, [rc_.res, t1.res], [rc_.res])
            ts(t1.ap, rc_.ap, -PI, 2 * PI, ALU.is_lt, ALU.mult, [rc_.res], [t1.res])
            tt(rc_.ap, rc_.ap, t1.ap, ALU.add, [rc_.res, t1.res], [rc_.res])
            ts(rc_.ap, rc_.ap, -3.14159, 3.14159, ALU.max, ALU.min, [rc_.res], [rc_.res])
            if which == 0:
                act(sin_ap, rc_.ap, AF.Sin, [rc_.res, ropec.res], [sin_res], scale=ropec.ap[:, 1:2])
            else:
                act(cos_ap, rc_.ap, AF.Sin, [rc_.res], [cos_res])

    xcnt = [0]

    def lowrank_norm(w3, wres, col0, x3, xres, gt, out3, out_res, t0, inv_c2, epscol):
        for fc in range(4):
            for kc in range(16):
                mm(psb(fc), w3[:, kc, col0 + fc * 128:col0 + (fc + 1) * 128], x3[:, kc, :], kc == 0, kc == 15,
                   [wres, xres], [pres[fc]])
            act(cf.ap[:, fc * 512:(fc + 1) * 512], psb(fc), AF.Copy, [pres[fc]], [cf.res])
            act(sq.ap[:, fc * 512:(fc + 1) * 512], psb(fc), AF.Square, [pres[fc]], [sq.res])
        for fc in range(4):
            mm(psb(4), ones_f.ap, sq.ap[:, fc * 512:(fc + 1) * 512], fc == 0, fc == 3, [ones_f.res, sq.res], [pres[4]])
        act(rs.ap, psb(4), AF.Sqrt, [pres[4], epsc.res], [rs.res], scale=inv_c2 / 512.0, bias=epsc.ap[:, epscol:epscol + 1])
        recip(rinv.ap, rs.ap, [rs.res], [rinv.res])
        for fc in range(4):
            stt(out3[:, fc, t0:t0 + 512], cf.ap[:, fc * 512:(fc + 1) * 512], gt.ap[:, fc:fc + 1], rinv.ap,
                ALU.mult, ALU.mult, [cf.res, gt.res, rinv.res], [out_res])

    def load_xblock(b):
        xt_ = xst[xcnt[0] % 2]
        xcnt[0] += 1
        src = xT_own[:, b * 512:(b + 1) * 512] if b < 4 else xT_oth[:, (b - 4) * 512:(b - 3) * 512]
        C.dma("pool", lambda: nc.gpsimd.dma_start(out=xt_.v3(16), in_=kp(src)), xt_.res, (), [xt_.res])
        return xt_

    for b in range(8):
        xt_ = load_xblock(b)
        prestage(2)
        x3 = xt_.v3(16)
        t0 = b * 512
        if b < 4:
            ca, cr, sa, sr = cos_o.ap[:, t0:t0 + 512], cos_o.res, sin_o.ap[:, t0:t0 + 512], sin_o.res
        else:
            ca, cr, sa, sr = cos_t.ap, cos_t.res, sin_t.ap, sin_t.res
        lowrank_norm(wkv3, wkv.res, 0, x3, xt_.res, kvg, ckvn3, ckvn.res, t0, 1.0, 1)
        for kc in range(16):
            mm(psb(5, 0, 512, 64), wkv3[:, kc, 512:576], x3[:, kc, :], kc == 0, kc == 15, [wkv.res, xt_.res], [pres[5]])
        for kc in range(16):
            mm(psb(6, 0, 512, 64), wkv3[:, kc, 576:640], x3[:, kc, :], kc == 0, kc == 15, [wkv.res, xt_.res], [pres[6]])
        rope_tables(t0, ca, cr, sa, sr)
        tt(t1.ap, psb(5, 0, 512, 64), ca, ALU.mult, [pres[5], cr], [t1.res])
        tt(t2.ap, psb(6, 0, 512, 64), sa, ALU.mult, [pres[6], sr], [t2.res])
        tt(kpe.ap[:, t0:t0 + 512], t1.ap, t2.ap, ALU.add, [t1.res, t2.res], [kpe.res])
    C.dma("pool", lambda: nc.gpsimd.dma_start(out=wkv3[:, :, 0:512], in_=kp(w_in[:, 3072:3584])), wkv.res, (), [wkv.res])
    for b in range(4):
        xt_ = load_xblock(b)
        prestage(2)
        lowrank_norm(wkv3, wkv.res, 0, xt_.v3(16), xt_.res, qg, cqn3, cqn.res, b * 512, 192.0, 2)

    pbarrier()
    sb.off = b_off
    wuq = sb.alloc(4 * 1536, BF16)
    wuqs = sb.alloc(4 * 512, BF16)
    wuk = sb.alloc(4 * 1024, BF16)
    wuv = sb.alloc(4 * 1024, BF16)
    qn = sb.alloc(2048, BF16)
    qpe = sb.alloc(2048, BF16, parts=64)
    kn = sb.alloc(4096, BF16)
    vh = sb.alloc(32 * 128, BF16)
    pring = [sb.alloc(512, BF16) for _ in range(4)]
    ppr = [sb.alloc(512, BF16) for _ in range(2)]
    u1 = sb.alloc(512, F32, parts=64)
    u2 = sb.alloc(512, F32, parts=64)
    rd2 = [sb.alloc(512, F32) for _ in range(2)]
    ost2 = [sb.alloc(512, BF16) for _ in range(2)]
    for (wt, src, k) in ((wuq, w_uq, 4), (wuqs, w_uqsw, 4), (wuk, w_uk, 4), (wuv, w_uv, 4)):
        C.dma("pool", lambda wt=wt, src=src: nc.gpsimd.dma_start(out=wt.v3(4), in_=kp(src)), wt.res, (), [wt.res])
    wuq3, wuqs3, wuk3, wuv3 = wuq.v3(4), wuqs.v3(4), wuk.v3(4), wuv.v3(4)

    step = 0
    qbc = 0
    for h in range(8):
        prestage(1)
        for blk in range(8):
            b = pbank()
            for rc in range(4):
                mm(psb(b), wuk3[:, rc, h * 128:(h + 1) * 128], ckvn3[:, rc, blk * 512:(blk + 1) * 512], rc == 0, rc == 3,
                   [wuk.res, ckvn.res], [pres[b]])
            act(kn.ap[:, blk * 512:(blk + 1) * 512], psb(b), AF.Copy, [pres[b]], [kn.res])
        for g0 in range(0, 32, 4):
            b = pbank()
            for gi in range(4):
                kc = g0 + gi
                for rc in range(4):
                    mm(psb(b, gi * 128, (gi + 1) * 128), ckvn3[:, rc, kc * 128:(kc + 1) * 128], wuv3[:, rc, h * 128:(h + 1) * 128],
                       rc == 0, rc == 3, [wuv.res, ckvn.res], [pres[b]])
            act(vh.ap[:, g0 * 128:(g0 + 4) * 128], psb(b), AF.Copy, [pres[b]], [vh.res])
        for blk in range(4):
            b = pbank()
            for rc in range(4):
                mm(psb(b), wuq3[:, rc, h * 192:h * 192 + 128], cqn3[:, rc, blk * 512:(blk + 1) * 512], rc == 0, rc == 3,
                   [wuq.res, cqn.res], [pres[b]])
            act(qn.ap[:, blk * 512:(blk + 1) * 512], psb(b), AF.Copy, [pres[b]], [qn.res])
            b1 = pbank()
            for rc in range(4):
                mm(psb(b1, 0, 512, 64), wuq3[:, rc, h * 192 + 128:h * 192 + 192], cqn3[:, rc, blk * 512:(blk + 1) * 512],
                   rc == 0, rc == 3, [wuq.res, cqn.res], [pres[b1]])
            tt(u1.ap, psb(b1, 0, 512, 64), cos_o.ap[:, blk * 512:(blk + 1) * 512], ALU.mult, [pres[b1], cos_o.res], [u1.res])
            b2 = pbank()
            for rc in range(4):
                mm(psb(b2, 0, 512, 64), wuqs3[:, rc, h * 64:(h + 1) * 64], cqn3[:, rc, blk * 512:(blk + 1) * 512],
                   rc == 0, rc == 3, [wuqs.res, cqn.res], [pres[b2]])
            tt(u2.ap, psb(b2, 0, 512, 64), sin_o.ap[:, blk * 512:(blk + 1) * 512], ALU.mult, [pres[b2], sin_o.res], [u2.res])
            tt(qpe.ap[:, blk * 512:(blk + 1) * 512], u1.ap, u2.ap, ALU.add, [u1.res, u2.res], [qpe.res])

        def emit_s(stp, qb, kc):
            sbk = stp % 2
            mm(psb(sbk), kn.ap[:, kc * 128:(kc + 1) * 128], qn.ap[:, qb * 512:(qb + 1) * 512], True, False,
               [kn.res, qn.res], [pres[sbk]])
            mm(psb(sbk), kpe.ap[:, kc * 128:(kc + 1) * 128], qpe.ap[:, qb * 512:(qb + 1) * 512], False, True,
               [kpe.res, qpe.res], [pres[sbk]])

        seq = [(qb, kc) for qb in range(4) for kc in range(32)]
        pend = []
        emit_s(step, *seq[0])
        for i, (qb, kc) in enumerate(seq):
            stp = step + i
            if i + 1 < len(seq):
                emit_s(stp + 1, *seq[i + 1])
            sbk = stp % 2
            pt = pring[stp % 4]
            if kc % 16 == 0:
                prestage(1)
            act(pt.ap, psb(sbk), AF.Exp, [pres[sbk]], [pt.res])
            ob = 2 + 2 * ((qbc + qb) % 2)
            mm(psb(ob), vh.ap[:, kc * 128:(kc + 1) * 128], pt.ap, kc == 0, kc == 31, [vh.res, pt.res], [pres[ob]])
            mm(psb(ob + 1), ones_b.ap, pt.ap, kc == 0, kc == 31, [ones_b.res, pt.res], [pres[ob + 1]])
            if kc == 31:
                rd = rd2[(qbc + qb) % 2]
                o2 = ost2[(qbc + qb) % 2]
                recip(rd.ap, psb(ob + 1), [pres[ob + 1]], [rd.res])
                tt(o2.ap, psb(ob), rd.ap, ALU.mult, [pres[ob], rd.res], [o2.res])
                st("sp", oT_d[8 + h][:, qb * 512:(qb + 1) * 512], o2.ap, o2.res, [o2.res])
        step += len(seq)
        qbc += 4

    pbarrier()
    sb.off = base_off

    wo = sb.alloc(16 * 2048, BF16)
    oTs = [sb.alloc(16 * 128, BF16) for _ in range(2)]
    xts = [sb.alloc(2048, F32) for _ in range(2)]
    y1s = [sb.alloc(2048, F32) for _ in range(2)]
    ln1g = sb.alloc(2048, F32)
    ln1b = sb.alloc(2048, F32)
    x1Ts = [sb.alloc(16 * 128, F32) for _ in range(2)]
    wrt = sb.alloc(16 * 72, F32)
    brt = sb.alloc(72, F32)
    statss = [sb.alloc(24, F32) for _ in range(2)]
    mvs = [sb.alloc(2, F32) for _ in range(2)]
    sds = [sb.alloc(1, F32) for _ in range(2)]
    rstds = [sb.alloc(1, F32) for _ in range(2)]
    nmrs = [sb.alloc(1, F32) for _ in range(2)]
    lgall = sb.alloc(16 * 72, F32)
    zt = sb.alloc(2048, F32)
    lgall3 = lgall.v3(16)

    wo3 = wo.v3(16)
    for g in range(4):
        C.dma("pool", lambda g=g: nc.gpsimd.dma_start(out=wo3[:, 4 * g:4 * g + 4, :], in_=kp(w_o)[:, 4 * g:4 * g + 4, :]),
              wo.res, (), [wo.res])
    ld("sp", ln1g.ap, ln1g_d, ln1g.res, [ln1g.res])
    ld("sp", ln1b.ap, ln1b_d, ln1b.res, [ln1b.res])
    ld("sp", wrt.v3(16), kp(w_rt), wrt.res, [wrt.res])
    ld("sp", brt.ap, b_rt, brt.res, [brt.res])
    wrt3 = wrt.v3(16)
    C.op("dve", lambda: nc.vector.memset(zt.ap, 0.0), (), [zt.res])
    st("sp", ys_d[NSLOT:NROWS, :], zt.ap, zt.res, [zt.res])

    def layer_norm(y, g_t, b_t, k, gb_eng="dve"):
        stats, mv, sd, rstd, nmr = statss[k], mvs[k], sds[k], rstds[k], nmrs[k]
        for i in range(4):
            C.op("dve", lambda i=i: nc.vector.bn_stats(out=stats.ap[:, i * 6:(i + 1) * 6], in_=y.ap[:, i * 512:(i + 1) * 512]),
                 [y.res], [stats.res])
        C.op("dve", lambda: nc.vector.bn_aggr(out=mv.ap, in_=stats.ap), [stats.res], [mv.res])
        act(sd.ap, mv.ap[:, 1:2], AF.Sqrt, [mv.res, epsc.res], [sd.res], bias=epsc.ap[:, 0:1])
        recip(rstd.ap, sd.ap, [sd.res], [rstd.res])
        stt(y.ap, y.ap, mv.ap[:, 0:1], g_t.ap, ALU.subtract, ALU.mult, [y.res, mv.res, g_t.res], [y.res])
        stt(y.ap, y.ap, rstd.ap, b_t.ap, ALU.mult, ALU.add, [y.res, rstd.res, b_t.res], [y.res])

    oT_v = oT_d.rearrange("c p t -> p c t")

    def c_loads(ti):
        ot, xt_ = oTs[ti % 2], xts[ti % 2]
        ld("sp", ot.v3(16), oT_v[:, :, ti * 128:ti * 128 + 128], ot.res, [ot.res])
        ld("sp", xt_.ap, x_own[ti * 128:ti * 128 + 128, :], xt_.res, [xt_.res])

    def c_p1(ti):
        ot, xt_, y1 = oTs[ti % 2], xts[ti % 2], y1s[ti % 2]
        ot3 = ot.v3(16)
        for nb in range(4):
            for fc in range(16):
                mm(psb(nb), ot3[:, fc, :], wo3[:, fc, nb * 512:(nb + 1) * 512], fc == 0, fc == 15,
                   [ot.res, wo.res], [pres[nb]])
            stt(y1.ap[:, nb * 512:(nb + 1) * 512], xt_.ap[:, nb * 512:(nb + 1) * 512], ALPHA, psb(nb), ALU.mult, ALU.add,
                [xt_.res, pres[nb]], [y1.res])

    c_loads(0)
    c_loads(1)
    c_p1(0)
    for tile_i in range(16):
        s = tile_i % 2
        r0 = tile_i * 128
        y1, x1T = y1s[s], x1Ts[s]
        layer_norm(y1, ln1g, ln1b, s)
        st("sp", x1s_d[r0:r0 + 128, :], y1.ap, y1.res, [y1.res])
        if tile_i + 1 < 16:
            c_p1(tile_i + 1)
        if tile_i + 2 < 16:
            c_loads(tile_i + 2)
        for kc in range(16):
            bb = 4 + kc // 4
            tr(psb(bb, (kc % 4) * 128, (kc % 4 + 1) * 128), y1.ap[:, kc * 128:(kc + 1) * 128], ident_f.ap,
               [y1.res, ident_f.res], [pres[bb]])
        for q in range(4):
            if q % 2 == 0:
                act(x1T.ap[:, q * 512:(q + 1) * 512], psb(4 + q), AF.Copy, [pres[4 + q]], [x1T.res])
            else:
                vcopy(x1T.ap[:, q * 512:(q + 1) * 512], psb(4 + q), [pres[4 + q]], [x1T.res])
        x1T3 = x1T.v3(16)
        for kc in range(16):
            mm(psb(4, 0, 72), x1T3[:, kc, :], wrt3[:, kc, :], kc == 0, kc == 15, [x1T.res, wrt.res], [pres[4]])
        tt(lgall3[:, tile_i, :], psb(4, 0, 72), brt.ap, ALU.add, [pres[4], brt.res], [lgall.res])

    NT = 16
    lgc = sb.alloc(NT * 8, F32)
    lec = sb.alloc(NT * 64, F32)
    gmax = sb.alloc(NT, F32)
    mg = sb.alloc(NT * 8, F32)
    gex = sb.alloc(NT * 8, F32)
    gsum = sb.alloc(NT, F32)
    gval = sb.alloc(NT, F32)
    pen = sb.alloc(NT * 8, F32)
    lem = sb.alloc(NT * 64, F32)
    m1 = sb.alloc(NT, F32)
    m2 = sb.alloc(NT, F32)
    oh1 = sb.alloc(NT * 64, F32)
    oh2 = sb.alloc(NT * 64, F32)
    dd = sb.alloc(NT, F32)
    ex = sb.alloc(NT, F32)
    w1 = sb.alloc(NT, F32)
    w2 = sb.alloc(NT, F32)
    Af = sb.alloc(NT * 64, F32)
    Ab = sb.alloc(NT * 64, BF16)
    cumt = sb.alloc(NT * 64, F32)
    smat = sb.alloc(NT * 64, F32)
    valid = sb.alloc(NT * 64, F32)
    tmpb = sb.alloc(NT * 64, F32)
    sv = [sb.alloc(NT, F32) for _ in range(4)]
    slf = [sb.alloc(NT, F32) for _ in range(2)]

    def v3(t_, k):
        return t_.ap.rearrange("p (t k) -> p t k", k=k)

    def bc(t_, k):
        return t_.ap.unsqueeze(2).broadcast_to([128, NT, k])

    vcopy(v3(lgc, 8), lgall3[:, :, 0:8], [lgall.res], [lgc.res])
    vcopy(v3(lec, 64), lgall3[:, :, 8:72], [lgall.res], [lec.res])
    C.op("dve", lambda: nc.vector.tensor_reduce(out=gmax.ap, in_=v3(lgc, 8), axis=AX.X, op=ALU.max), [lgc.res], [gmax.res])
    tt(v3(mg, 8), v3(lgc, 8), bc(gmax, 8), ALU.is_equal, [lgc.res, gmax.res], [mg.res])
    tt(v3(gex, 8), v3(lgc, 8), bc(gmax, 8), ALU.subtract, [lgc.res, gmax.res], [gex.res])
    act(gex.ap, gex.ap, AF.Exp, [gex.res], [gex.res])
    C.op("dve", lambda: nc.vector.tensor_reduce(out=gsum.ap, in_=v3(gex, 8), axis=AX.X, op=ALU.add), [gex.res], [gsum.res])
    recip(gval.ap, gsum.ap, [gsum.res], [gval.res])
    ts(pen.ap, mg.ap, 1.0, 1e30, ALU.subtract, ALU.mult, [mg.res], [pen.res])
    tt(lem.ap.rearrange("p (t g k) -> p t g k", g=8, k=8), lec.ap.rearrange("p (t g k) -> p t g k", g=8, k=8),
       pen.ap.rearrange("p (t g) -> p t g", g=8).unsqueeze(3).broadcast_to([128, NT, 8, 8]), ALU.add,
       [lec.res, pen.res], [lem.res])
    C.op("dve", lambda: nc.vector.tensor_reduce(out=m1.ap, in_=v3(lem, 64), axis=AX.X, op=ALU.max), [lem.res], [m1.res])
    tt(v3(oh1, 64), v3(lem, 64), bc(m1, 64), ALU.is_equal, [lem.res, m1.res], [oh1.res])
    stt(lem.ap, oh1.ap, -1e30, lem.ap, ALU.mult, ALU.add, [oh1.res, lem.res], [lem.res])
    C.op("dve", lambda: nc.vector.tensor_reduce(out=m2.ap, in_=v3(lem, 64), axis=AX.X, op=ALU.max), [lem.res], [m2.res])
    tt(v3(oh2, 64), v3(lem, 64), bc(m2, 64), ALU.is_equal, [lem.res, m2.res], [oh2.res])
    tt(dd.ap, m2.ap, m1.ap, ALU.subtract, [m1.res, m2.res], [dd.res])
    act(ex.ap, dd.ap, AF.Exp, [dd.res], [ex.res])
    ts(w1.ap, ex.ap, 1.0, None, ALU.add, None, [ex.res], [w1.res])
    recip(w1.ap, w1.ap, [w1.res], [w1.res])
    tt(w2.ap, ex.ap, w1.ap, ALU.mult, [ex.res, w1.res], [w2.res])
    tt(Af.ap, oh1.ap, oh2.ap, ALU.add, [oh1.res, oh2.res], [Af.res])
    vcopy(Ab.ap, Af.ap, [Af.res], [Ab.res])
    for hb in range(2):
        mm(psb(hb), tri_b.ap, Ab.ap[:, hb * 512:(hb + 1) * 512], True, True, [tri_b.res, Ab.res], [pres[hb]])
        mm(psb(2 + hb), ones_b.ap, Ab.ap[:, hb * 512:(hb + 1) * 512], True, True, [ones_b.res, Ab.res], [pres[2 + hb]])
    C.op("dve", lambda: nc.vector.memset(cumt.ap[:, 0:64], 0.0), (), [cumt.res])
    for t in range(1, NT):
        pb_, pc_ = 2 + (t - 1) // 8, ((t - 1) % 8) * 64
        tt(cumt.ap[:, t * 64:(t + 1) * 64], cumt.ap[:, (t - 1) * 64:t * 64], psb(pb_, pc_, pc_ + 64), ALU.add,
           [cumt.res, pres[pb_]], [cumt.res])
    for hb in range(2):
        tt(smat.ap[:, hb * 512:(hb + 1) * 512], psb(hb), cumt.ap[:, hb * 512:(hb + 1) * 512], ALU.add,
           [pres[hb], cumt.res], [smat.res])
    ts(valid.ap, smat.ap, 127.5, None, ALU.is_lt, None, [smat.res], [valid.res])
    tt(v3(smat, 64), v3(smat, 64), ebase.ap.unsqueeze(1).broadcast_to([128, NT, 64]), ALU.add, [smat.res, ebase.res], [smat.res])
    for k, oh in enumerate((oh1, oh2)):
        s_t, v_t = sv[2 * k], sv[2 * k + 1]
        tt(tmpb.ap, oh.ap, smat.ap, ALU.mult, [oh.res, smat.res], [tmpb.res])
        C.op("dve", lambda: nc.vector.tensor_reduce(out=s_t.ap, in_=v3(tmpb, 64), axis=AX.X, op=ALU.add), [tmpb.res], [s_t.res])
        tt(tmpb.ap, oh.ap, valid.ap, ALU.mult, [oh.res, valid.res], [tmpb.res])
        C.op("dve", lambda: nc.vector.tensor_reduce(out=v_t.ap, in_=v3(tmpb, 64), axis=AX.X, op=ALU.add), [tmpb.res], [v_t.res])
        sl = slf[k]
        ts(sl.ap, s_t.ap, dummyc.ap, None, ALU.subtract, None, [s_t.res, dummyc.res], [sl.res])
        tt(sl.ap, sl.ap, v_t.ap, ALU.mult, [sl.res, v_t.res], [sl.res])
        ts(sl.ap, sl.ap, dummyc.ap, None, ALU.add, None, [sl.res, dummyc.res], [sl.res])
        sl_t, g_t, w_t = (slot1_i, g1s, w1) if k == 0 else (slot2_i, g2s, w2)
        vcopy(sl_t.ap, sl.ap, [sl.res], [sl_t.res])
        tt(g_t.ap, gval.ap, w_t.ap, ALU.mult, [gval.res, w_t.res], [g_t.res])
        tt(g_t.ap, g_t.ap, v_t.ap, ALU.mult, [g_t.res, v_t.res], [g_t.res])
    if debug:
        st("sp", rt_d, Af.ap[:, 0:64], Af.res, [Af.res])

    pbarrier()
    sb.off = base_off
    x1f = [sb.alloc(2048, F32) for _ in range(2)]
    x1b = [sb.alloc(2048, BF16) for _ in range(2)]

    def s_loads(ti):
        ld("sp", x1f[ti % 2].ap, x1s_d[ti * 128:ti * 128 + 128, :], x1f[ti % 2].res, [x1f[ti % 2].res])

    s_loads(0)
    for tile_i in range(16):
        if tile_i + 1 < 16:
            s_loads(tile_i + 1)
        xf, xb = x1f[tile_i % 2], x1b[tile_i % 2]
        if tile_i % 2 == 0:
            act(xb.ap, xf.ap, AF.Copy, [xf.res], [xb.res])
        else:
            vcopy(xb.ap, xf.ap, [xf.res], [xb.res])
        for sl_t in (slot1_i, slot2_i):
            C.dma("pool", lambda sl_t=sl_t, xb=xb: nc.gpsimd.indirect_dma_start(
                out=xs_d, out_offset=bass.IndirectOffsetOnAxis(ap=sl_t.ap[:, tile_i:tile_i + 1], axis=0),
                in_=xb.ap, in_offset=None, bounds_check=BR, oob_is_err=False),
                xb.res, [xb.res, sl_t.res], ())

    C.barrier()
    sb.off = base_off

    wgs = [sb.alloc(16 * 512, BF16) for _ in range(2)]
    wus = [sb.alloc(16 * 512, BF16) for _ in range(2)]
    wds = [sb.alloc(4 * 2048, BF16) for _ in range(2)]
    xgs = [sb.alloc(2048, BF16) for _ in range(2)]
    xgTs = [sb.alloc(2048, BF16) for _ in range(2)]
    sg = sb.alloc(512, F32)
    hdn = [sb.alloc(512, BF16) for _ in range(2)]
    hdnT = [sb.alloc(512, BF16) for _ in range(2)]
    ysts = [sb.alloc(2048, F32) for _ in range(2)]
    ycnt = 0

    def d_loads(e):
        wg, wu, wd, xg = wgs[e % 2], wus[e % 2], wds[e % 2], xgs[e % 2]
        ld("sp", xg.ap, xs_d[e * 128:(e + 1) * 128, :], xg.res, [xg.res])
        if PRESTAGE and e < NPRE:
            ld("sp", wg.ap, wgb_d[e], wg.res, [wg.res])
            ld("sp", wu.ap, wub_d[e], wu.res, [wu.res])
            ld("sp", wd.ap, wdb_d[e], wd.res, [wd.res])
        else:
            C.dma("pool", lambda: nc.gpsimd.dma_start(out=wg.v3(16), in_=kp(w_gate[e])), wg.res, (), [wg.res])
            C.dma("pool", lambda: nc.gpsimd.dma_start(out=wu.v3(16), in_=kp(w_up[e])), wu.res, (), [wu.res])
            C.dma("pool", lambda: nc.gpsimd.dma_start(out=wd.v3(4), in_=kp(w_down[e])), wd.res, (), [wd.res])

    d_loads(0)
    for e in range(64):
        s = e % 2
        wg, wu, wd, xg, xgT, hd, hT, yst = wgs[s], wus[s], wds[s], xgs[s], xgTs[s], hdn[s], hdnT[s], ysts[s]
        if e + 1 < 64:
            d_loads(e + 1)
        wg3, wu3, wd3 = wg.v3(16), wu.v3(16), wd.v3(4)
        for kc in range(16):
            bb = kc // 8
            tr(psbf(bb, (kc % 8) * 128, (kc % 8 + 1) * 128), xg.ap[:, kc * 128:(kc + 1) * 128], ident_b.ap,
               [xg.res, ident_b.res], [pres[bb]])
        act(xgT.ap[:, 0:1024], psbf(0, 0, 1024), AF.Copy, [pres[0]], [xgT.res])
        vcopy(xgT.ap[:, 1024:2048], psbf(1, 0, 1024), [pres[1]], [xgT.res])
        for kc in range(16):
            mm(psb(2), xgT.ap[:, kc * 128:(kc + 1) * 128], wg3[:, kc, :], kc == 0, kc == 15, [xgT.res, wg.res], [pres[2]])
        for kc in range(16):
            mm(psb(3), xgT.ap[:, kc * 128:(kc + 1) * 128], wu3[:, kc, :], kc == 0, kc == 15, [xgT.res, wu.res], [pres[3]])
        act(sg.ap, psb(2), AF.Silu, [pres[2]], [sg.res])
        tt(hd.ap, psb(3), sg.ap, ALU.mult, [pres[3], sg.res], [hd.res])
        for fc in range(4):
            tr(psbf(4, fc * 128, (fc + 1) * 128), hd.ap[:, fc * 128:(fc + 1) * 128], ident_b.ap, [hd.res, ident_b.res], [pres[4]])
        vcopy(hT.ap, psbf(4, 0, 512), [pres[4]], [hT.res])
        for nb in range(4):
            yb = 5 + ycnt % 3
            ycnt += 1
            for fc in range(4):
                mm(psb(yb), hT.ap[:, fc * 128:(fc + 1) * 128], wd3[:, fc, nb * 512:(nb + 1) * 512], fc == 0, fc == 3,
                   [hT.res, wd.res], [pres[yb]])
            if nb % 2 == 0:
                act(yst.ap[:, nb * 512:(nb + 1) * 512], psb(yb), AF.Copy, [pres[yb]], [yst.res])
            else:
                vcopy(yst.ap[:, nb * 512:(nb + 1) * 512], psb(yb), [pres[yb]], [yst.res])
        st("sp", ys_d[e * 128:(e + 1) * 128, :], yst.ap, yst.res, [yst.res])

    C.barrier()
    sb.off = base_off

    wpg = sb.alloc(16 * 2048, BF16)
    wple = sb.alloc(2 * 2048, BF16)
    pTt = sb.alloc(2 * 2048, BF16)
    ln2g = sb.alloc(2048, F32)
    ln2b = sb.alloc(2048, F32)
    pleg = sb.alloc(2048, F32)
    yas = [sb.alloc(2048, F32) for _ in range(3)]
    ybs = [sb.alloc(2048, F32) for _ in range(3)]
    zs = [sb.alloc(2048, F32) for _ in range(3)]
    gates = ybs
    x2Ts = [sb.alloc(2048, BF16) for _ in range(2)]
    statss = [sb.alloc(24, F32) for _ in range(2)]
    mvs = [sb.alloc(2, F32) for _ in range(2)]
    sds = [sb.alloc(1, F32) for _ in range(2)]
    rstds = [sb.alloc(1, F32) for _ in range(2)]
    nmrs = [sb.alloc(1, F32) for _ in range(2)]
    sss = [sb.alloc(1, F32) for _ in range(2)]
    sd2s = [sb.alloc(1, F32) for _ in range(2)]
    rr2s = [sb.alloc(1, F32) for _ in range(2)]
    wpg3 = wpg.v3(16)
    for g in range(4):
        C.dma("pool", lambda g=g: nc.gpsimd.dma_start(out=wpg3[:, 4 * g:4 * g + 4, :], in_=kp(w_pg)[:, 4 * g:4 * g + 4, :]),
              wpg.res, (), [wpg.res])
    C.dma("pool", lambda: nc.gpsimd.dma_start(out=wple.v3(2), in_=kp(w_ple)), wple.res, (), [wple.res])
    C.dma("pool", lambda: nc.gpsimd.dma_start(out=pTt.v3(2), in_=kp(pT_d)), pTt.res, (), [pTt.res])
    ld("sp", ln2g.ap, ln2g_d, ln2g.res, [ln2g.res])
    ld("sp", ln2b.ap, ln2b_d, ln2b.res, [ln2b.res])
    ld("sp", pleg.ap, pleg_d, pleg.res, [pleg.res])
    wple3, pTt3 = wple.v3(2), pTt.v3(2)

    def e_loads(ti):
        k = ti % 3
        for (yt_, sl_t) in ((yas[k], slot1_i), (ybs[k], slot2_i)):
            C.dma("pool", lambda yt_=yt_, sl_t=sl_t: nc.gpsimd.indirect_dma_start(
                out=yt_.ap, out_offset=None, in_=ys_d,
                in_offset=bass.IndirectOffsetOnAxis(ap=sl_t.ap[:, ti:ti + 1], axis=0),
                bounds_check=BR, oob_is_err=False), yt_.res, [sl_t.res], [yt_.res])
        ld("sp", zs[k].ap, x1s_d[ti * 128:ti * 128 + 128, :], zs[k].res, [zs[k].res])

    def e_s1(tile_i):
        k = tile_i % 2
        k3 = tile_i % 3
        ya, yb_, z = yas[k3], ybs[k3], zs[k3]
        act(z.ap, z.ap, AF.Copy, [z.res], [z.res], scale=ALPHA)
        stt(z.ap, ya.ap, g1s.ap[:, tile_i:tile_i + 1], z.ap, ALU.mult, ALU.add, [ya.res, g1s.res, z.res], [z.res])
        stt(z.ap, yb_.ap, g2s.ap[:, tile_i:tile_i + 1], z.ap, ALU.mult, ALU.add, [yb_.res, g2s.res, z.res], [z.res])
        layer_norm(z, ln2g, ln2b, k)

    def e_s2a(tile_i):
        k = tile_i % 2
        k3 = tile_i % 3
        z, x2T = zs[k3], x2Ts[k]
        for kc in range(16):
            bb = 4 + kc // 4
            tr(psb(bb, (kc % 4) * 128, (kc % 4 + 1) * 128), z.ap[:, kc * 128:(kc + 1) * 128], ident_f.ap,
               [z.res, ident_f.res], [pres[bb]])
        for q in range(4):
            if q % 2 == 0:
                act(x2T.ap[:, q * 512:(q + 1) * 512], psb(4 + q), AF.Copy, [pres[4 + q]], [x2T.res])
            else:
                vcopy(x2T.ap[:, q * 512:(q + 1) * 512], psb(4 + q), [pres[4 + q]], [x2T.res])

    def e_s2b(tile_i):
        k = tile_i % 2
        k3 = tile_i % 3
        r0 = tile_i * 128
        ya, yb_, z, gate, x2T, ss, sd2, rr2 = yas[k3], ybs[k3], zs[k3], gates[k3], x2Ts[k], sss[k], sd2s[k], rr2s[k]
        for nb in range(4):
            for kc in range(16):
                mm(psb(nb), x2T.ap[:, kc * 128:(kc + 1) * 128], wpg3[:, kc, nb * 512:(nb + 1) * 512], kc == 0, kc == 15,
                   [x2T.res, wpg.res], [pres[nb]])
            act(gate.ap[:, nb * 512:(nb + 1) * 512], psb(nb), AF.Sigmoid, [pres[nb]], [gate.res])
        for nb in range(4):
            for qc in range(2):
                mm(psb(4 + nb), pTt3[:, qc, r0:r0 + 128], wple3[:, qc, nb * 512:(nb + 1) * 512], qc == 0, qc == 1,
                   [pTt.res, wple.res], [pres[4 + nb]])
            tt(gate.ap[:, nb * 512:(nb + 1) * 512], psb(4 + nb), gate.ap[:, nb * 512:(nb + 1) * 512], ALU.mult,
               [pres[4 + nb], gate.res], [gate.res])

    def e_s3(tile_i):
        k = tile_i % 2
        k3 = tile_i % 3
        r0 = tile_i * 128
        ya, yb_, z, gate, x2T, ss, sd2, rr2 = yas[k3], ybs[k3], zs[k3], gates[k3], x2Ts[k], sss[k], sd2s[k], rr2s[k]
        act(ya.ap, gate.ap, AF.Square, [gate.res], [ya.res, ss.res], accum_out=ss.ap)
        act(sd2.ap, ss.ap, AF.Sqrt, [ss.res, epsc.res], [sd2.res], scale=1.0 / D, bias=epsc.ap[:, 1:2])
        recip(rr2.ap, sd2.ap, [sd2.res], [rr2.res])
        stt(gate.ap, gate.ap, rr2.ap, pleg.ap, ALU.mult, ALU.mult, [gate.res, rr2.res, pleg.res], [gate.res])
        tt(gate.ap, gate.ap, z.ap, ALU.add, [gate.res, z.res], [gate.res])
        st("sp", out_d[r0:r0 + 128, :], gate.ap, gate.res, [gate.res])

    e_loads(0)
    e_loads(1)
    e_loads(2)
    e_s1(0)
    e_s2a(0)
    for tile_i in range(16):
        if tile_i + 1 < 16:
            e_s1(tile_i + 1)
        e_s2b(tile_i)
        if tile_i + 1 < 16:
            e_s2a(tile_i + 1)
        e_s3(tile_i)
        if tile_i + 3 < 16:
            e_loads(tile_i + 3)

    C.barrier()
    es.close()
    return nc, C


_CACHE = {}


def _consts():
    ident = np.eye(128, dtype=np.float32)
    tri = np.triu(np.ones((128, 128), np.float32), 1)
    inv_freq = (1.0 / (10000.0 ** (np.arange(0, 64, 2, dtype=np.float32) / 64.0))).astype(np.float32)
    ropec = np.zeros((64, 4), np.float32)
    ropec[:, 0] = np.concatenate([inv_freq, inv_freq])
    ropec[:32, 1] = -1.0
    ropec[32:, 1] = 1.0
    ebase = np.broadcast_to((np.arange(64, dtype=np.float32) * 128.0)[None, :], (128, 64)).copy()
    dummy = (NSLOT + np.arange(128, dtype=np.float32)).reshape(128, 1)
    return ident, tri, ropec, ebase, dummy


def _build_natab(rpb, half):
    tab = np.full((8, 5, 128, 7, 128), -30000.0, np.float32)
    own_row0 = 32 * half
    qc = np.arange(64)
    c0 = np.clip(qc - 8, 0, 48)
    for ty in range(5):
        jl = {0: 0, 1: 1, 2: 2, 3: 14, 4: 15}[ty]
        for qr in range(2):
            r = own_row0 + 2 * jl + qr
            start = min(max(r - 4, 0), 56)
            for ci in range(7):
                for kr in range(2):
                    krow = own_row0 + 2 * (jl + ci - 3) + kr
                    if krow < start or krow > start + 7 or krow < 0 or krow > 63:
                        continue
                    ro = krow - r + 7
                    for w in range(16):
                        kcs = c0 + w
                        vals = rpb[:, ro, kcs - qc + 15]
                        tab[:, ty, kr * 64 + kcs, ci, qr * 64 + qc] = vals
    return tab.reshape(8, 5, 128, 896)


def kernel(x, p, positions, w_in, rpb, q_norm_g, kv_norm_g, w_uq, w_uk, w_uv, w_o,
           ln1_g, ln1_b, w_group, b_group, w_router, b_router, w_gate, w_up, w_down,
           ln2_g, ln2_b, w_ple, w_ple_gate, ple_norm_g):
    f = lambda a: np.ascontiguousarray(np.asarray(a, dtype=np.float32))
    x = f(x); p = f(p); w_in0 = f(w_in)[0]
    positions = np.asarray(positions).astype(np.int32)
    debug = True
    if ("nc", debug) not in _CACHE:
        _CACHE[("nc", debug)] = build_program(debug)[0]
    nc = _CACHE[("nc", debug)]
    ident, tri, ropec, ebase, dummy = _consts()
    rep = lambda v: np.ascontiguousarray(np.broadcast_to(f(v).reshape(1, -1), (128, f(v).size)))
    s5 = 3 * 1024 + 1024
    w_krsw = np.ascontiguousarray(np.concatenate([w_in0[:, s5 + 32:s5 + 64], w_in0[:, s5:s5 + 32]], axis=1))
    wuq0 = f(w_uq)[0]
    wuq_h = wuq0.reshape(512, 8, 192)
    w_uqsw = np.ascontiguousarray(np.concatenate([wuq_h[:, :, 160:192], wuq_h[:, :, 128:160]], axis=2).reshape(512, 512))
    w_rt = np.ascontiguousarray(np.concatenate([f(w_group)[0], f(w_router)[0]], axis=1))
    b_rt = rep(np.concatenate([f(b_group)[0].reshape(-1), f(b_router)[0].reshape(-1)]))
    shared = {
        "w_in": w_in0, "w_krsw": w_krsw, "w_uq": wuq0, "w_uqsw": w_uqsw, "w_uk": f(w_uk)[0], "w_uv": f(w_uv)[0],
        "qg": np.ascontiguousarray(f(q_norm_g)[0].reshape(4, 128).T), "kvg": np.ascontiguousarray(f(kv_norm_g)[0].reshape(4, 128).T),
        "w_o": f(w_o)[0], "ln1g": rep(f(ln1_g)[0]), "ln1b": rep(f(ln1_b)[0]), "ln2g": rep(f(ln2_g)[0]), "ln2b": rep(f(ln2_b)[0]),
        "pleg": rep(f(ple_norm_g)[0]), "w_rt": w_rt, "b_rt": b_rt, "w_gate": f(w_gate)[0], "w_up": f(w_up)[0],
        "w_down": f(w_down)[0], "w_ple": f(w_ple)[0], "w_pg": f(w_ple_gate)[0],
        "ident": ident, "tri": tri, "ropec": ropec, "ebase": ebase, "dummyc": dummy,
    }
    rpb0 = f(rpb)[0]
    natabs = [_build_natab(rpb0, 0), _build_natab(rpb0, 1)]
    in_maps = []
    for c in range(8):
        b, half = c // 2, c % 2
        own = x[b, half * 2048:(half + 1) * 2048]
        oth = x[b, (1 - half) * 2048:(2 - half) * 2048]
        halo = np.zeros((768, D), np.float32)
        if half == 0:
            halo[384:768] = oth[0:384]
        else:
            halo[0:384] = oth[2048 - 384:2048]
        pos = np.concatenate([positions[b, half * 2048:(half + 1) * 2048], positions[b, (1 - half) * 2048:(2 - half) * 2048]])
        m = dict(shared)
        m.update({
            "xT_own": np.ascontiguousarray(own.T), "xT_oth": np.ascontiguousarray(oth.T),
            "xT_halo": np.ascontiguousarray(halo.T), "x_own": np.ascontiguousarray(own),
            "pT": np.ascontiguousarray(p[0, b, half * 2048:(half + 1) * 2048].T),
            "posb": np.ascontiguousarray(np.broadcast_to(pos[None, :], (64, 4096))).astype(np.int32),
            "natab": natabs[half],
        })
        in_maps.append(m)
    res = run_bass_kernel_spmd(nc, in_maps, core_ids=list(range(8)))
    out = np.empty((4, 4096, D), np.float32)
    for c in range(8):
        b, half = c // 2, c % 2
        out[b, half * 2048:(half + 1) * 2048] = res.results[c]["out"]
    if debug:
        _CACHE["dbg"] = res.results
    return out
```

```python
import math
import os
import numpy as np
from contextlib import ExitStack
import concourse.bass as bass
import concourse.mybir as mybir
from concourse.bass_utils import run_bass_kernel_spmd

F32 = mybir.dt.float32
BF16 = mybir.dt.bfloat16
I32 = mybir.dt.int32
U8 = mybir.dt.uint8
AF = mybir.ActivationFunctionType
ALU = mybir.AluOpType
AX = mybir.AxisListType
DTSIZE = {F32: 4, BF16: 2, I32: 4, U8: 1}

D = 2048
NTOK = 2048
ALPHA = 2.0 ** 0.25
LN_EPS = 1e-5
RMS_EPS = 1e-6
NSLOT = 8192
NROWS = NSLOT + 128
PRESTAGE = False
NPRE = 32
SBUF_CAP = 200 * 1024
PI = math.pi


class SemRec:
    def __init__(self, h):
        self.h = h
        self.count = 0


class Res:
    __slots__ = ("w", "r_eng", "r_dma", "dsem")

    def __init__(self):
        self.w = None
        self.r_eng = {}
        self.r_dma = {}
        self.dsem = None


class Eng:
    def __init__(self, name, h, sem):
        self.name = name
        self.h = h
        self.sem = sem
        self.count = 0
        self.known = {}
        self.knownd = {}


class T:
    def __init__(self, ap):
        self.ap = ap
        self.res = Res()

    def v3(self, k):
        return self.ap.rearrange("p (k f) -> p k f", k=k)


class Ctx:
    def __init__(self, nc, es):
        self.nc = nc
        self.es = es
        self.engs = {}
        for name, h in (("pe", nc.tensor), ("act", nc.scalar), ("dve", nc.vector),
                        ("pool", nc.gpsimd), ("sp", nc.sync)):
            sem = SemRec(es.enter_context(nc.semaphore("e_" + name)))
            self.engs[name] = Eng(name, h, sem)
        self.dsems = []
        self.n_inst = 0

    def new_dsem(self, name):
        s = SemRec(self.es.enter_context(self.nc.semaphore(name)))
        self.dsems.append(s)
        return s

    def _wait(self, eng, tok):
        if tok[0] == "e":
            src, c = tok[1], tok[2]
            if src is eng and eng.name == "pe":
                return
            if eng.known.get(src.name, 0) >= c:
                return
            eng.h.wait_ge(src.sem.h, c)
            eng.known[src.name] = c
        else:
            s = tok[1]
            v = s.count
            if eng.knownd.get(id(s), 0) >= v:
                return
            eng.h.wait_ge(s.h, v)
            eng.knownd[id(s)] = v

    def _deps(self, eng, reads, writes):
        for r in reads:
            if r.w is not None:
                self._wait(eng, r.w)
        for w in writes:
            if w.w is not None:
                self._wait(eng, w.w)
            for en, c in w.r_eng.items():
                self._wait(eng, ("e", self.engs[en], c))
            for s in w.r_dma.values():
                self._wait(eng, ("d", s))

    def op(self, ename, fn, reads=(), writes=()):
        eng = self.engs[ename]
        self._deps(eng, reads, writes)
        inst = fn()
        eng.count += 1
        inst.then_inc(eng.sem.h, 1)
        eng.known[eng.name] = max(eng.known.get(eng.name, 0), 0)
        tok = ("e", eng, eng.count)
        for r in reads:
            r.r_eng[eng.name] = eng.count
        for w in writes:
            w.w = tok
            w.r_eng = {}
            w.r_dma = {}
        self.n_inst += 1

    def dma(self, q, fn, semres, reads=(), writes=()):
        eng = self.engs[q]
        self._deps(eng, reads, writes)
        if semres.dsem is None:
            semres.dsem = self.new_dsem("d%d" % len(self.dsems))
        s = semres.dsem
        inst = fn()
        s.count += 16
        inst.then_inc(s.h, 16)
        tok = ("d", s)
        for r in reads:
            r.r_dma[id(s)] = s
        for w in writes:
            w.w = tok
            w.r_eng = {}
            w.r_dma = {}
        self.n_inst += 1

    def barrier(self, exclude=()):
        for e in self.engs.values():
            for o in self.engs.values():
                if o is not e and o.count > 0:
                    self._wait(e, ("e", o, o.count))
            for s in self.dsems:
                if s.count > 0 and s not in exclude:
                    self._wait(e, ("d", s))


class SB:
    def __init__(self, big, cap):
        self.big = big
        self.cap = cap
        self.off = 0

    def alloc(self, cols, dtype, parts=128):
        size = cols * DTSIZE[dtype]
        size = (size + 63) // 64 * 64
        assert self.off + size <= self.cap, ("SBUF overflow", self.off, size, self.cap)
        ap = self.big[0:parts, self.off:self.off + size]
        if dtype != U8:
            ap = ap.bitcast(dtype)
        ap = ap[:, 0:cols]
        self.off += size
        return T(ap)


def build_program(debug=False):
    nc = bass.Bass("TRN2", target_bir_lowering=False)
    es = ExitStack()

    def din(name, shape, dt=F32):
        return nc.dram_tensor(name, list(shape), dt, kind="ExternalInput").ap()

    xT_own = din("xT_own", [D, NTOK])
    xT_oth = din("xT_oth", [D, NTOK])
    xT_halo = din("xT_halo", [D, 768])
    x_own = din("x_own", [NTOK, D])
    pT_d = din("pT", [256, NTOK])
    posb = din("posb", [64, 4096], I32)
    w_in = din("w_in", [D, 4160])
    w_krsw = din("w_krsw", [D, 64])
    w_uq = din("w_uq", [512, 1536])
    w_uqsw = din("w_uqsw", [512, 512])
    w_uk = din("w_uk", [512, 1024])
    w_uv = din("w_uv", [512, 1024])
    qg_d = din("qg", [128, 4])
    kvg_d = din("kvg", [128, 4])
    w_o = din("w_o", [D, D])
    ln1g_d = din("ln1g", [128, D])
    ln1b_d = din("ln1b", [128, D])
    ln2g_d = din("ln2g", [128, D])
    ln2b_d = din("ln2b", [128, D])
    pleg_d = din("pleg", [128, D])
    w_rt = din("w_rt", [D, 72])
    b_rt = din("b_rt", [128, 72])
    w_gate = din("w_gate", [64, D, 512])
    w_up = din("w_up", [64, D, 512])
    w_down = din("w_down", [64, 512, D])
    w_ple = din("w_ple", [256, D])
    w_pg = din("w_pg", [D, D])
    natab = din("natab", [8, 5, 128, 896])
    ident_d = din("ident", [128, 128])
    tri_d = din("tri", [128, 128])
    ropec_d = din("ropec", [64, 4])
    ebase_d = din("ebase", [128, 64])
    dummy_d = din("dummyc", [128, 1])

    out_d = nc.dram_tensor("out", [NTOK, D], F32, kind="ExternalOutput").ap()
    skind = "ExternalOutput" if debug else "Internal"
    oT_d = nc.dram_tensor("oT_s", [16, 128, NTOK], BF16, kind=skind).ap()
    x1s_d = nc.dram_tensor("x1_s", [NTOK, D], F32, kind=skind).ap()
    xs_d = nc.dram_tensor("xs_s", [NROWS, D], BF16, kind="Internal").ap()
    ys_d = nc.dram_tensor("ys_s", [NROWS, D], F32, kind="Internal").ap()
    rt_d = nc.dram_tensor("rt_s", [128, 64], F32, kind=skind).ap()
    if PRESTAGE:
        wgb_d = nc.dram_tensor("wgb_s", [NPRE, 128, 16 * 512], BF16, kind="Internal").ap()
        wub_d = nc.dram_tensor("wub_s", [NPRE, 128, 16 * 512], BF16, kind="Internal").ap()
        wdb_d = nc.dram_tensor("wdb_s", [NPRE, 128, 4 * D], BF16, kind="Internal").ap()

    big = es.enter_context(nc.sbuf_tensor("big", [128, SBUF_CAP], U8))
    ps = es.enter_context(nc.psum_tensor("ps", [128, 8, 512], F32))
    C = Ctx(nc, es)
    sb = SB(big, SBUF_CAP)
    pres = [Res() for _ in range(8)]
    BR = nc.gpsimd.to_reg(NROWS - 1)

    def psb(b, c0=0, c1=512, parts=128):
        return ps[0:parts, b, c0:c1]

    def psbf(b, c0, c1):
        return ps[:, b, :].bitcast(BF16)[:, c0:c1]

    def mm(out, lhsT, rhs, start, stop, reads, writes):
        C.op("pe", lambda: nc.tensor.matmul(out, lhsT, rhs, start=start, stop=stop), reads, writes)

    def tr(out, in_, ident, reads, writes):
        C.op("pe", lambda: nc.tensor.transpose(out, in_, ident), reads, writes)

    def act(out, in_, func, reads, writes, **kw):
        C.op("act", lambda: nc.scalar.activation(out=out, in_=in_, func=func, **kw), reads, writes)

    def tt(out, in0, in1, op, reads, writes):
        C.op("dve", lambda: nc.vector.tensor_tensor(out=out, in0=in0, in1=in1, op=op), reads, writes)

    def ts(out, in0, s1, s2, op0, op1, reads, writes):
        if s2 is None:
            C.op("dve", lambda: nc.vector.tensor_scalar(out=out, in0=in0, scalar1=s1, scalar2=None, op0=op0),
                 reads, writes)
        else:
            C.op("dve", lambda: nc.vector.tensor_scalar(out=out, in0=in0, scalar1=s1, scalar2=s2, op0=op0, op1=op1),
                 reads, writes)

    def stt(out, in0, scalar, in1, op0, op1, reads, writes):
        C.op("dve", lambda: nc.vector.scalar_tensor_tensor(out=out, in0=in0, scalar=scalar, in1=in1, op0=op0, op1=op1),
             reads, writes)

    def vcopy(out, in_, reads, writes):
        C.op("dve", lambda: nc.vector.tensor_copy(out=out, in_=in_), reads, writes)

    def recip(out, in_, reads, writes):
        C.op("dve", lambda: nc.vector.reciprocal(out=out, in_=in_), reads, writes)

    def rmax(out, in_, reads, writes):
        C.op("dve", lambda: nc.vector.reduce_max(out=out, in_=in_, axis=AX.X), reads, writes)

    def rsum(out, in_, reads, writes):
        C.op("dve", lambda: nc.vector.reduce_sum(out=out, in_=in_, axis=AX.X), reads, writes)

    def ld(q, t_ap, d_ap, semres, writes):
        C.dma(q, lambda: C.engs[q].h.dma_start(out=t_ap, in_=d_ap), semres, (), writes)

    def st(q, d_ap, t_ap, semres, reads):
        C.dma(q, lambda: C.engs[q].h.dma_start(out=d_ap, in_=t_ap), semres, reads, ())

    def kp(dram2d, p=128):
        return dram2d.rearrange("(k p) f -> p k f", p=p)

    ident_f = sb.alloc(128, F32)
    ident_b = sb.alloc(128, BF16)
    ones_b = sb.alloc(128, BF16)
    ones_f = sb.alloc(128, F32)
    tri_b = sb.alloc(128, BF16)
    ropec = sb.alloc(4, F32, parts=64)
    ebase = sb.alloc(64, F32)
    dummyc = sb.alloc(1, F32)
    slot1_i = sb.alloc(16, I32)
    slot2_i = sb.alloc(16, I32)
    g1s = sb.alloc(16, F32)
    g2s = sb.alloc(16, F32)
    cum = sb.alloc(64, F32)
    epsc = sb.alloc(4, F32)
    ld("sp", ident_f.ap, ident_d, ident_f.res, [ident_f.res])
    ld("pool", ident_b.ap, ident_d, ident_b.res, [ident_b.res])
    ld("pool", tri_b.ap, tri_d, tri_b.res, [tri_b.res])
    ld("sp", ropec.ap, ropec_d, ropec.res, [ropec.res])
    ld("sp", ebase.ap, ebase_d, ebase.res, [ebase.res])
    ld("sp", dummyc.ap, dummy_d, dummyc.res, [dummyc.res])
    C.op("dve", lambda: nc.vector.memset(ones_b.ap, 1.0), (), [ones_b.res])
    C.op("dve", lambda: nc.vector.memset(ones_f.ap, 1.0), (), [ones_f.res])
    C.op("dve", lambda: nc.vector.memset(cum.ap, 0.0), (), [cum.res])
    C.op("dve", lambda: nc.vector.memset(epsc.ap[:, 0:1], LN_EPS), (), [epsc.res])
    C.op("dve", lambda: nc.vector.memset(epsc.ap[:, 1:2], RMS_EPS), (), [epsc.res])
    C.op("dve", lambda: nc.vector.memset(epsc.ap[:, 2:3], RMS_EPS * 192.0), (), [epsc.res])
    C.op("dve", lambda: nc.vector.memset(epsc.ap[:, 3:4], 0.0), (), [epsc.res])
    base_off = sb.off

    pst = Res()
    pst_items = []
    for e_ in range(NPRE if PRESTAGE else 0):
        pst_items.append((wgb_d[e_].rearrange("p (k f) -> p k f", k=16), w_gate[e_].rearrange("(k p) f -> p k f", p=128)))
        pst_items.append((wub_d[e_].rearrange("p (k f) -> p k f", k=16), w_up[e_].rearrange("(k p) f -> p k f", p=128)))
        pst_items.append((wdb_d[e_].rearrange("p (k f) -> p k f", k=4), w_down[e_].rearrange("(k p) f -> p k f", p=128)))
    pst_i = [0]

    def prestage(n):
        if not PRESTAGE:
            return
        for _ in range(n):
            if pst_i[0] >= len(pst_items):
                return
            o_, i_ = pst_items[pst_i[0]]
            pst_i[0] += 1
            C.dma("pool", lambda: nc.gpsimd.dma_start(out=o_, in_=i_), pst, (), ())

    def pbarrier():
        C.barrier(exclude=(pst.dsem,) if pst.dsem is not None else ())

    xo = sb.alloc(16 * 2048, BF16)
    xh = sb.alloc(16 * 768, BF16)
    wqs = [sb.alloc(16 * 128, BF16) for _ in range(2)]
    wks = [sb.alloc(16 * 128, BF16) for _ in range(2)]
    wvs = [sb.alloc(16 * 128, BF16) for _ in range(2)]
    qT = sb.alloc(2048, BF16)
    kT = sb.alloc(22 * 128, BF16)
    vN = sb.alloc(22 * 128, BF16)
    tabs = [sb.alloc(896, F32) for _ in range(2)]
    scs = [sb.alloc(896, F32) for _ in range(2)]
    pTs = [sb.alloc(896, BF16) for _ in range(2)]
    rdn = [sb.alloc(128, F32) for _ in range(2)]
    ost = [sb.alloc(2048, BF16) for _ in range(2)]

    xo3 = xo.v3(16)
    xh3 = xh.v3(16)
    for g in range(4):
        C.dma("pool", lambda g=g: nc.gpsimd.dma_start(out=xo3[:, 4 * g:4 * g + 4, :], in_=kp(xT_own)[:, 4 * g:4 * g + 4, :]),
              xo.res, (), [xo.res])
    C.dma("pool", lambda: nc.gpsimd.dma_start(out=xh3, in_=kp(xT_halo)), xh.res, (), [xh.res])

    def ext_src(e):
        if e < 3:
            return xh, xh3, e * 128
        if e < 19:
            return xo, xo3, (e - 3) * 128
        return xh, xh3, (e - 19 + 3) * 128

    tab_loaded = [0]
    tab_list = [(h, ty) for h in range(8) for ty in range(5)]

    def ensure_tab(li):
        while tab_loaded[0] <= min(li + 1, len(tab_list) - 1):
            i = tab_loaded[0]
            hh, ty = tab_list[i]
            t = tabs[i % 2]
            ld("sp", t.ap, natab[hh, ty], t.res, [t.res])
            tab_loaded[0] += 1

    pcnt = [0]

    def pbank():
        pcnt[0] += 1
        return 6 + (pcnt[0] % 2)

    na_scale = 128.0 ** -0.5
    ucnt = 0
    for h in range(8):
        s = h % 2
        for (wt, c0) in ((wqs[s], h * 128), (wks[s], 1024 + h * 128), (wvs[s], 2048 + h * 128)):
            C.dma("pool", lambda wt=wt, c0=c0: nc.gpsimd.dma_start(out=wt.v3(16), in_=kp(w_in[:, c0:c0 + 128])),
                  wt.res, (), [wt.res])
        wq3, wk3, wv3 = wqs[s].v3(16), wks[s].v3(16), wvs[s].v3(16)
        for tb in range(4):
            b = pbank()
            for kc in range(16):
                mm(psb(b), wq3[:, kc, :], xo3[:, kc, tb * 512:(tb + 1) * 512], kc == 0, kc == 15,
                   [wqs[s].res, xo.res], [pres[b]])
            act(qT.ap[:, tb * 512:(tb + 1) * 512], psb(b), AF.Copy, [pres[b]], [qT.res], scale=na_scale)
        kblocks = [(xh, xh3, 0, 384, 0)] + [(xo, xo3, tb * 512, 512, 384 + tb * 512) for tb in range(4)] + \
                  [(xh, xh3, 384, 384, 384 + 2048)]
        for (xt_, x3, c0, n, o0) in kblocks:
            b = pbank()
            for kc in range(16):
                mm(psb(b, 0, n), wk3[:, kc, :], x3[:, kc, c0:c0 + n], kc == 0, kc == 15,
                   [wks[s].res, xt_.res], [pres[b]])
            act(kT.ap[:, o0:o0 + n], psb(b, 0, n), AF.Copy, [pres[b]], [kT.res])
        for g0 in range(0, 22, 4):
            n = min(4, 22 - g0)
            b = pbank()
            for gi in range(n):
                xt_, x3, c0 = ext_src(g0 + gi)
                for kc in range(16):
                    mm(psb(b, gi * 128, (gi + 1) * 128), x3[:, kc, c0:c0 + 128], wv3[:, kc, :], kc == 0, kc == 15,
                       [wvs[s].res, xt_.res], [pres[b]])
            act(vN.ap[:, g0 * 128:(g0 + n) * 128], psb(b, 0, n * 128), AF.Copy, [pres[b]], [vN.res])
        o_t = ost[h % 2]

        def na_S(j, u):
            b0 = 2 * u
            for ci in range(7):
                e = j + ci
                bb, cc = (b0, ci * 128) if ci < 4 else (b0 + 1, (ci - 4) * 128)
                mm(psb(bb, cc, cc + 128), kT.ap[:, e * 128:(e + 1) * 128], qT.ap[:, j * 128:(j + 1) * 128], True, True,
                   [kT.res, qT.res], [pres[bb]])

        def na_softmax(j, u):
            ty = 0 if j == 0 else 1 if j == 1 else 2 if j < 14 else j - 11
            li = h * 5 + ty
            ensure_tab(li)
            tb_ = tabs[li % 2]
            b0 = 2 * u
            sc, pt = scs[u], pTs[u]
            tt(sc.ap[:, 0:512], psb(b0), tb_.ap[:, 0:512], ALU.add, [pres[b0], tb_.res], [sc.res])
            tt(sc.ap[:, 512:896], psb(b0 + 1, 0, 384), tb_.ap[:, 512:896], ALU.add, [pres[b0 + 1], tb_.res], [sc.res])
            act(pt.ap, sc.ap, AF.Exp, [sc.res], [pt.res])

        na_S(0, ucnt % 2)
        na_softmax(0, ucnt % 2)
        for j in range(16):
            u = ucnt % 2
            ucnt += 1
            ob = 4 + u
            if j + 1 < 16:
                na_S(j + 1, 1 - u)
                na_softmax(j + 1, 1 - u)
            pt = pTs[u]
            for ci in range(7):
                e = j + ci
                mm(psb(ob, 0, 128), vN.ap[:, e * 128:(e + 1) * 128], pt.ap[:, ci * 128:(ci + 1) * 128], ci == 0, ci == 6,
                   [vN.res, pt.res], [pres[ob]])
            for ci in range(7):
                mm(psb(ob, 256, 384), ones_b.ap, pt.ap[:, ci * 128:(ci + 1) * 128], ci == 0, ci == 6,
                   [ones_b.res, pt.res], [pres[ob]])
            rd = rdn[u]
            recip(rd.ap, psb(ob, 256, 384), [pres[ob]], [rd.res])
            tt(o_t.ap[:, j * 128:(j + 1) * 128], psb(ob, 0, 128), rd.ap, ALU.mult, [pres[ob], rd.res], [o_t.res])
        st("sp", oT_d[h], o_t.ap, o_t.res, [o_t.res])

    pbarrier()
    sb.off = base_off

    cqn = sb.alloc(4 * 2048, BF16)
    ckvn = sb.alloc(4 * 4096, BF16)
    kpe = sb.alloc(4096, BF16, parts=64)
    cos_o = sb.alloc(2048, F32, parts=64)
    sin_o = sb.alloc(2048, F32, parts=64)
    qg = sb.alloc(4, F32)
    kvg = sb.alloc(4, F32)
    ld("sp", qg.ap, qg_d, qg.res, [qg.res])
    ld("sp", kvg.ap, kvg_d, kvg.res, [kvg.res])
    cqn3 = cqn.v3(4)
    ckvn3 = ckvn.v3(4)
    b_off = sb.off

    wkv = sb.alloc(16 * 640, BF16)
    xst = [sb.alloc(16 * 512, BF16) for _ in range(3)]
    cf = sb.alloc(4 * 512, F32)
    sq = sb.alloc(4 * 512, F32)
    rs = sb.alloc(512, F32)
    rinv = sb.alloc(512, F32)
    posi = sb.alloc(512, I32, parts=64)
    ra = sb.alloc(512, F32, parts=64)
    rb = sb.alloc(512, F32, parts=64)
    rki = sb.alloc(512, I32, parts=64)
    rc_ = sb.alloc(512, F32, parts=64)
    cos_t = sb.alloc(512, F32, parts=64)
    sin_t = sb.alloc(512, F32, parts=64)
    t1 = sb.alloc(512, F32, parts=64)
    t2 = sb.alloc(512, F32, parts=64)
    wkv3 = wkv.v3(16)
    C.dma("pool", lambda: nc.gpsimd.dma_start(out=wkv3[:, :, 0:576], in_=kp(w_in[:, 3584:4160])), wkv.res, (), [wkv.res])
    C.dma("pool", lambda: nc.gpsimd.dma_start(out=wkv3[:, :, 576:640], in_=kp(w_krsw)), wkv.res, (), [wkv.res])

    def rope_tables(tok0, cos_ap, cos_res, sin_ap, sin_res):
        ld("sp", posi.ap, posb[:, tok0:tok0 + 512], posi.res, [posi.res])
        vcopy(ra.ap, posi.ap, [posi.res], [ra.res])
        ts(ra.ap, ra.ap, ropec.ap[:, 0:1], None, ALU.mult, None, [ra.res, ropec.res], [ra.res])
        for which in (0, 1):
            if which == 1:
                ts(rb.ap, ra.ap, PI / 2, None, ALU.add, None, [ra.res], [rb.res])
                src = rb
            else:
                src = ra
            ts(rc_.ap, src.ap, 1.0 / (2 * PI), None, ALU.mult, None, [src.res], [rc_.res])
            vcopy(rki.ap, rc_.ap, [rc_.res], [rki.res])
            vcopy(rc_.ap, rki.ap, [rki.res], [rc_.res])
            stt(rc_.ap, rc_.ap, -2 * PI, src.ap, ALU.mult, ALU.add, [rc_.res, src.res], [rc_.res])
            ts(t1.ap, rc_.ap, PI, -2 * PI, ALU.is_gt, ALU.mult, [rc_.res], [t1.res])
            tt(rc_.ap, rc_.ap, t1.ap, ALU.add, [rc_.res, t1.res], [rc_.res])
            ts(t1.ap, rc_.ap, -PI, 2 * PI, ALU.is_lt, ALU.mult, [rc_.res], [t1.res])
            tt(rc_.ap, rc_.ap, t1.ap, ALU.add, [rc_.res, t1.res], [rc_.res])
            ts(rc_.ap, rc_.ap, -3.14159, 3.14159, ALU.max, ALU.min, [rc_.res], [rc_.res])
            if which == 0:
                act(sin_ap, rc_.ap, AF.Sin, [rc_.res, ropec.res], [sin_res], scale=ropec.ap[:, 1:2])
            else:
                act(cos_ap, rc_.ap, AF.Sin, [rc_.res], [cos_res])

    xcnt = [0]

    def lowrank_norm(w3, wres, col0, x3, xres, gt, out3, out_res, t0, inv_c2, epscol):
        for fc in range(4):
            for kc in range(16):
                mm(psb(fc), w3[:, kc, col0 + fc * 128:col0 + (fc + 1) * 128], x3[:, kc, :], kc == 0, kc == 15,
                   [wres, xres], [pres[fc]])
            act(cf.ap[:, fc * 512:(fc + 1) * 512], psb(fc), AF.Copy, [pres[fc]], [cf.res])
            act(sq.ap[:, fc * 512:(fc + 1) * 512], psb(fc), AF.Square, [pres[fc]], [sq.res])
        for fc in range(4):
            mm(psb(4), ones_f.ap, sq.ap[:, fc * 512:(fc + 1) * 512], fc == 0, fc == 3, [ones_f.res, sq.res], [pres[4]])
        act(rs.ap, psb(4), AF.Sqrt, [pres[4], epsc.res], [rs.res], scale=inv_c2 / 512.0, bias=epsc.ap[:, epscol:epscol + 1])
        recip(rinv.ap, rs.ap, [rs.res], [rinv.res])
        for fc in range(4):
            stt(out3[:, fc, t0:t0 + 512], cf.ap[:, fc * 512:(fc + 1) * 512], gt.ap[:, fc:fc + 1], rinv.ap,
                ALU.mult, ALU.mult, [cf.res, gt.res, rinv.res], [out_res])

    def load_xblock(b):
        xt_ = xst[xcnt[0] % 3]
        xcnt[0] += 1
        src = xT_own[:, b * 512:(b + 1) * 512] if b < 4 else xT_oth[:, (b - 4) * 512:(b - 3) * 512]
        C.dma("pool", lambda: nc.gpsimd.dma_start(out=xt_.v3(16), in_=kp(src)), xt_.res, (), [xt_.res])
        return xt_

    xq = [load_xblock(0)]
    for b in range(8):
        xt_ = xq.pop(0)
        xq.append(load_xblock(b + 1) if b + 1 < 8 else None)
        prestage(2)
        x3 = xt_.v3(16)
        t0 = b * 512
        if b < 4:
            ca, cr, sa, sr = cos_o.ap[:, t0:t0 + 512], cos_o.res, sin_o.ap[:, t0:t0 + 512], sin_o.res
        else:
            ca, cr, sa, sr = cos_t.ap, cos_t.res, sin_t.ap, sin_t.res
        lowrank_norm(wkv3, wkv.res, 0, x3, xt_.res, kvg, ckvn3, ckvn.res, t0, 1.0, 1)
        for kc in range(16):
            mm(psb(5, 0, 512, 64), wkv3[:, kc, 512:576], x3[:, kc, :], kc == 0, kc == 15, [wkv.res, xt_.res], [pres[5]])
        for kc in range(16):
            mm(psb(6, 0, 512, 64), wkv3[:, kc, 576:640], x3[:, kc, :], kc == 0, kc == 15, [wkv.res, xt_.res], [pres[6]])
        rope_tables(t0, ca, cr, sa, sr)
        tt(t1.ap, psb(5, 0, 512, 64), ca, ALU.mult, [pres[5], cr], [t1.res])
        tt(t2.ap, psb(6, 0, 512, 64), sa, ALU.mult, [pres[6], sr], [t2.res])
        tt(kpe.ap[:, t0:t0 + 512], t1.ap, t2.ap, ALU.add, [t1.res, t2.res], [kpe.res])
    C.dma("pool", lambda: nc.gpsimd.dma_start(out=wkv3[:, :, 0:512], in_=kp(w_in[:, 3072:3584])), wkv.res, (), [wkv.res])
    for b in range(4):
        xt_ = load_xblock(b)
        prestage(2)
        lowrank_norm(wkv3, wkv.res, 0, xt_.v3(16), xt_.res, qg, cqn3, cqn.res, b * 512, 192.0, 2)

    pbarrier()
    sb.off = b_off
    wuq = sb.alloc(4 * 1536, BF16)
    wuqs = sb.alloc(4 * 512, BF16)
    wuk = sb.alloc(4 * 1024, BF16)
    wuv = sb.alloc(4 * 1024, BF16)
    qn = sb.alloc(2048, BF16)
    qpe = sb.alloc(2048, BF16, parts=64)
    kn = sb.alloc(4096, BF16)
    vh = sb.alloc(32 * 128, BF16)
    pring = [sb.alloc(512, BF16) for _ in range(4)]
    ppr = [sb.alloc(512, BF16) for _ in range(2)]
    u1 = sb.alloc(512, F32, parts=64)
    u2 = sb.alloc(512, F32, parts=64)
    rd2 = [sb.alloc(512, F32) for _ in range(2)]
    ost2 = [sb.alloc(512, BF16) for _ in range(2)]
    for (wt, src, k) in ((wuq, w_uq, 4), (wuqs, w_uqsw, 4), (wuk, w_uk, 4), (wuv, w_uv, 4)):
        C.dma("pool", lambda wt=wt, src=src: nc.gpsimd.dma_start(out=wt.v3(4), in_=kp(src)), wt.res, (), [wt.res])
    wuq3, wuqs3, wuk3, wuv3 = wuq.v3(4), wuqs.v3(4), wuk.v3(4), wuv.v3(4)

    step = 0
    qbc = 0
    for h in range(8):
        prestage(1)
        for blk in range(8):
            b = pbank()
            for rc in range(4):
                mm(psb(b), wuk3[:, rc, h * 128:(h + 1) * 128], ckvn3[:, rc, blk * 512:(blk + 1) * 512], rc == 0, rc == 3,
                   [wuk.res, ckvn.res], [pres[b]])
            act(kn.ap[:, blk * 512:(blk + 1) * 512], psb(b), AF.Copy, [pres[b]], [kn.res])
        for g0 in range(0, 32, 4):
            b = pbank()
            for gi in range(4):
                kc = g0 + gi
                for rc in range(4):
                    mm(psb(b, gi * 128, (gi + 1) * 128), ckvn3[:, rc, kc * 128:(kc + 1) * 128], wuv3[:, rc, h * 128:(h + 1) * 128],
                       rc == 0, rc == 3, [wuv.res, ckvn.res], [pres[b]])
            act(vh.ap[:, g0 * 128:(g0 + 4) * 128], psb(b), AF.Copy, [pres[b]], [vh.res])
        for blk in range(4):
            b = pbank()
            for rc in range(4):
                mm(psb(b), wuq3[:, rc, h * 192:h * 192 + 128], cqn3[:, rc, blk * 512:(blk + 1) * 512], rc == 0, rc == 3,
                   [wuq.res, cqn.res], [pres[b]])
            act(qn.ap[:, blk * 512:(blk + 1) * 512], psb(b), AF.Copy, [pres[b]], [qn.res])
            b1 = pbank()
            for rc in range(4):
                mm(psb(b1, 0, 512, 64), wuq3[:, rc, h * 192 + 128:h * 192 + 192], cqn3[:, rc, blk * 512:(blk + 1) * 512],
                   rc == 0, rc == 3, [wuq.res, cqn.res], [pres[b1]])
            tt(u1.ap, psb(b1, 0, 512, 64), cos_o.ap[:, blk * 512:(blk + 1) * 512], ALU.mult, [pres[b1], cos_o.res], [u1.res])
            b2 = pbank()
            for rc in range(4):
                mm(psb(b2, 0, 512, 64), wuqs3[:, rc, h * 64:(h + 1) * 64], cqn3[:, rc, blk * 512:(blk + 1) * 512],
                   rc == 0, rc == 3, [wuqs.res, cqn.res], [pres[b2]])
            tt(u2.ap, psb(b2, 0, 512, 64), sin_o.ap[:, blk * 512:(blk + 1) * 512], ALU.mult, [pres[b2], sin_o.res], [u2.res])
            tt(qpe.ap[:, blk * 512:(blk + 1) * 512], u1.ap, u2.ap, ALU.add, [u1.res, u2.res], [qpe.res])

        def emit_s(stp, qb, kc):
            sbk = stp % 2
            mm(psb(sbk), kn.ap[:, kc * 128:(kc + 1) * 128], qn.ap[:, qb * 512:(qb + 1) * 512], True, False,
               [kn.res, qn.res], [pres[sbk]])
            mm(psb(sbk), kpe.ap[:, kc * 128:(kc + 1) * 128], qpe.ap[:, qb * 512:(qb + 1) * 512], False, True,
               [kpe.res, qpe.res], [pres[sbk]])

        seq = [(qb, kc) for qb in range(4) for kc in range(32)]
        pend = []
        emit_s(step, *seq[0])
        for i, (qb, kc) in enumerate(seq):
            stp = step + i
            if i + 1 < len(seq):
                emit_s(stp + 1, *seq[i + 1])
            sbk = stp % 2
            pt = pring[stp % 4]
            if kc % 16 == 0:
                prestage(1)
            act(pt.ap, psb(sbk), AF.Exp, [pres[sbk]], [pt.res])
            ob = 2 + 2 * ((qbc + qb) % 2)
            mm(psb(ob), vh.ap[:, kc * 128:(kc + 1) * 128], pt.ap, kc == 0, kc == 31, [vh.res, pt.res], [pres[ob]])
            mm(psb(ob + 1), ones_b.ap, pt.ap, kc == 0, kc == 31, [ones_b.res, pt.res], [pres[ob + 1]])
            if kc == 31:
                rd = rd2[(qbc + qb) % 2]
                o2 = ost2[(qbc + qb) % 2]
                recip(rd.ap, psb(ob + 1), [pres[ob + 1]], [rd.res])
                tt(o2.ap, psb(ob), rd.ap, ALU.mult, [pres[ob], rd.res], [o2.res])
                st("sp", oT_d[8 + h][:, qb * 512:(qb + 1) * 512], o2.ap, o2.res, [o2.res])
        step += len(seq)
        qbc += 4

    pbarrier()
    sb.off = base_off

    wo = sb.alloc(16 * 2048, BF16)
    oTs = [sb.alloc(16 * 128, BF16) for _ in range(2)]
    xts = [sb.alloc(2048, F32) for _ in range(2)]
    y1s = [sb.alloc(2048, F32) for _ in range(2)]
    ln1g = sb.alloc(2048, F32)
    ln1b = sb.alloc(2048, F32)
    x1Ts = [sb.alloc(16 * 128, F32) for _ in range(2)]
    wrt = sb.alloc(16 * 72, F32)
    brt = sb.alloc(72, F32)
    statss = [sb.alloc(24, F32) for _ in range(2)]
    mvs = [sb.alloc(2, F32) for _ in range(2)]
    sds = [sb.alloc(1, F32) for _ in range(2)]
    rstds = [sb.alloc(1, F32) for _ in range(2)]
    nmrs = [sb.alloc(1, F32) for _ in range(2)]
    lgall = sb.alloc(16 * 72, F32)
    zt = sb.alloc(2048, F32)
    lgall3 = lgall.v3(16)

    wo3 = wo.v3(16)
    for g in range(4):
        C.dma("pool", lambda g=g: nc.gpsimd.dma_start(out=wo3[:, 4 * g:4 * g + 4, :], in_=kp(w_o)[:, 4 * g:4 * g + 4, :]),
              wo.res, (), [wo.res])
    ld("sp", ln1g.ap, ln1g_d, ln1g.res, [ln1g.res])
    ld("sp", ln1b.ap, ln1b_d, ln1b.res, [ln1b.res])
    ld("sp", wrt.v3(16), kp(w_rt), wrt.res, [wrt.res])
    ld("sp", brt.ap, b_rt, brt.res, [brt.res])
    wrt3 = wrt.v3(16)
    C.op("dve", lambda: nc.vector.memset(zt.ap, 0.0), (), [zt.res])
    st("sp", ys_d[NSLOT:NROWS, :], zt.ap, zt.res, [zt.res])

    def layer_norm(y, g_t, b_t, k, gb_eng="dve"):
        stats, mv, sd, rstd, nmr = statss[k], mvs[k], sds[k], rstds[k], nmrs[k]
        for i in range(4):
            C.op("dve", lambda i=i: nc.vector.bn_stats(out=stats.ap[:, i * 6:(i + 1) * 6], in_=y.ap[:, i * 512:(i + 1) * 512]),
                 [y.res], [stats.res])
        C.op("dve", lambda: nc.vector.bn_aggr(out=mv.ap, in_=stats.ap), [stats.res], [mv.res])
        act(sd.ap, mv.ap[:, 1:2], AF.Sqrt, [mv.res, epsc.res], [sd.res], bias=epsc.ap[:, 0:1])
        recip(rstd.ap, sd.ap, [sd.res], [rstd.res])
        stt(y.ap, y.ap, mv.ap[:, 0:1], g_t.ap, ALU.subtract, ALU.mult, [y.res, mv.res, g_t.res], [y.res])
        stt(y.ap, y.ap, rstd.ap, b_t.ap, ALU.mult, ALU.add, [y.res, rstd.res, b_t.res], [y.res])

    oT_v = oT_d.rearrange("c p t -> p c t")

    def c_loads(ti):
        ot, xt_ = oTs[ti % 2], xts[ti % 2]
        ld("sp", ot.v3(16), oT_v[:, :, ti * 128:ti * 128 + 128], ot.res, [ot.res])
        ld("sp", xt_.ap, x_own[ti * 128:ti * 128 + 128, :], xt_.res, [xt_.res])

    def c_p1(ti):
        ot, xt_, y1 = oTs[ti % 2], xts[ti % 2], y1s[ti % 2]
        ot3 = ot.v3(16)
        for nb in range(4):
            for fc in range(16):
                mm(psb(nb), ot3[:, fc, :], wo3[:, fc, nb * 512:(nb + 1) * 512], fc == 0, fc == 15,
                   [ot.res, wo.res], [pres[nb]])
            stt(y1.ap[:, nb * 512:(nb + 1) * 512], xt_.ap[:, nb * 512:(nb + 1) * 512], ALPHA, psb(nb), ALU.mult, ALU.add,
                [xt_.res, pres[nb]], [y1.res])

    c_loads(0)
    c_loads(1)
    c_p1(0)
    for tile_i in range(16):
        s = tile_i % 2
        r0 = tile_i * 128
        y1, x1T = y1s[s], x1Ts[s]
        layer_norm(y1, ln1g, ln1b, s)
        st("sp", x1s_d[r0:r0 + 128, :], y1.ap, y1.res, [y1.res])
        if tile_i + 1 < 16:
            c_p1(tile_i + 1)
        if tile_i + 2 < 16:
            c_loads(tile_i + 2)
        for kc in range(16):
            bb = 4 + kc // 4
            tr(psb(bb, (kc % 4) * 128, (kc % 4 + 1) * 128), y1.ap[:, kc * 128:(kc + 1) * 128], ident_f.ap,
               [y1.res, ident_f.res], [pres[bb]])
        for q in range(4):
            if q % 2 == 0:
                act(x1T.ap[:, q * 512:(q + 1) * 512], psb(4 + q), AF.Copy, [pres[4 + q]], [x1T.res])
            else:
                vcopy(x1T.ap[:, q * 512:(q + 1) * 512], psb(4 + q), [pres[4 + q]], [x1T.res])
        x1T3 = x1T.v3(16)
        for kc in range(16):
            mm(psb(4, 0, 72), x1T3[:, kc, :], wrt3[:, kc, :], kc == 0, kc == 15, [x1T.res, wrt.res], [pres[4]])
        tt(lgall3[:, tile_i, :], psb(4, 0, 72), brt.ap, ALU.add, [pres[4], brt.res], [lgall.res])

    NT = 16
    lgc = sb.alloc(NT * 8, F32)
    lec = sb.alloc(NT * 64, F32)
    gmax = sb.alloc(NT, F32)
    mg = sb.alloc(NT * 8, F32)
    gex = sb.alloc(NT * 8, F32)
    gsum = sb.alloc(NT, F32)
    gval = sb.alloc(NT, F32)
    pen = sb.alloc(NT * 8, F32)
    lem = sb.alloc(NT * 64, F32)
    m1 = sb.alloc(NT, F32)
    m2 = sb.alloc(NT, F32)
    oh1 = sb.alloc(NT * 64, F32)
    oh2 = sb.alloc(NT * 64, F32)
    dd = sb.alloc(NT, F32)
    ex = sb.alloc(NT, F32)
    w1 = sb.alloc(NT, F32)
    w2 = sb.alloc(NT, F32)
    Af = sb.alloc(NT * 64, F32)
    Ab = sb.alloc(NT * 64, BF16)
    cumt = sb.alloc(NT * 64, F32)
    smat = sb.alloc(NT * 64, F32)
    valid = sb.alloc(NT * 64, F32)
    tmpb = sb.alloc(NT * 64, F32)
    sv = [sb.alloc(NT, F32) for _ in range(4)]
    slf = [sb.alloc(NT, F32) for _ in range(2)]

    def v3(t_, k):
        return t_.ap.rearrange("p (t k) -> p t k", k=k)

    def bc(t_, k):
        return t_.ap.unsqueeze(2).broadcast_to([128, NT, k])

    vcopy(v3(lgc, 8), lgall3[:, :, 0:8], [lgall.res], [lgc.res])
    vcopy(v3(lec, 64), lgall3[:, :, 8:72], [lgall.res], [lec.res])
    C.op("dve", lambda: nc.vector.tensor_reduce(out=gmax.ap, in_=v3(lgc, 8), axis=AX.X, op=ALU.max), [lgc.res], [gmax.res])
    tt(v3(mg, 8), v3(lgc, 8), bc(gmax, 8), ALU.is_equal, [lgc.res, gmax.res], [mg.res])
    tt(v3(gex, 8), v3(lgc, 8), bc(gmax, 8), ALU.subtract, [lgc.res, gmax.res], [gex.res])
    act(gex.ap, gex.ap, AF.Exp, [gex.res], [gex.res])
    C.op("dve", lambda: nc.vector.tensor_reduce(out=gsum.ap, in_=v3(gex, 8), axis=AX.X, op=ALU.add), [gex.res], [gsum.res])
    recip(gval.ap, gsum.ap, [gsum.res], [gval.res])
    ts(pen.ap, mg.ap, 1.0, 1e30, ALU.subtract, ALU.mult, [mg.res], [pen.res])
    tt(lem.ap.rearrange("p (t g k) -> p t g k", g=8, k=8), lec.ap.rearrange("p (t g k) -> p t g k", g=8, k=8),
       pen.ap.rearrange("p (t g) -> p t g", g=8).unsqueeze(3).broadcast_to([128, NT, 8, 8]), ALU.add,
       [lec.res, pen.res], [lem.res])
    C.op("dve", lambda: nc.vector.tensor_reduce(out=m1.ap, in_=v3(lem, 64), axis=AX.X, op=ALU.max), [lem.res], [m1.res])
    tt(v3(oh1, 64), v3(lem, 64), bc(m1, 64), ALU.is_equal, [lem.res, m1.res], [oh1.res])
    stt(lem.ap, oh1.ap, -1e30, lem.ap, ALU.mult, ALU.add, [oh1.res, lem.res], [lem.res])
    C.op("dve", lambda: nc.vector.tensor_reduce(out=m2.ap, in_=v3(lem, 64), axis=AX.X, op=ALU.max), [lem.res], [m2.res])
    tt(v3(oh2, 64), v3(lem, 64), bc(m2, 64), ALU.is_equal, [lem.res, m2.res], [oh2.res])
    tt(dd.ap, m2.ap, m1.ap, ALU.subtract, [m1.res, m2.res], [dd.res])
    act(ex.ap, dd.ap, AF.Exp, [dd.res], [ex.res])
    ts(w1.ap, ex.ap, 1.0, None, ALU.add, None, [ex.res], [w1.res])
    recip(w1.ap, w1.ap, [w1.res], [w1.res])
    tt(w2.ap, ex.ap, w1.ap, ALU.mult, [ex.res, w1.res], [w2.res])
    tt(Af.ap, oh1.ap, oh2.ap, ALU.add, [oh1.res, oh2.res], [Af.res])
    vcopy(Ab.ap, Af.ap, [Af.res], [Ab.res])
    for hb in range(2):
        mm(psb(hb), tri_b.ap, Ab.ap[:, hb * 512:(hb + 1) * 512], True, True, [tri_b.res, Ab.res], [pres[hb]])
        mm(psb(2 + hb), ones_b.ap, Ab.ap[:, hb * 512:(hb + 1) * 512], True, True, [ones_b.res, Ab.res], [pres[2 + hb]])
    C.op("dve", lambda: nc.vector.memset(cumt.ap[:, 0:64], 0.0), (), [cumt.res])
    for t in range(1, NT):
        pb_, pc_ = 2 + (t - 1) // 8, ((t - 1) % 8) * 64
        tt(cumt.ap[:, t * 64:(t + 1) * 64], cumt.ap[:, (t - 1) * 64:t * 64], psb(pb_, pc_, pc_ + 64), ALU.add,
           [cumt.res, pres[pb_]], [cumt.res])
    for hb in range(2):
        tt(smat.ap[:, hb * 512:(hb + 1) * 512], psb(hb), cumt.ap[:, hb * 512:(hb + 1) * 512], ALU.add,
           [pres[hb], cumt.res], [smat.res])
    ts(valid.ap, smat.ap, 127.5, None, ALU.is_lt, None, [smat.res], [valid.res])
    tt(v3(smat, 64), v3(smat, 64), ebase.ap.unsqueeze(1).broadcast_to([128, NT, 64]), ALU.add, [smat.res, ebase.res], [smat.res])
    for k, oh in enumerate((oh1, oh2)):
        s_t, v_t = sv[2 * k], sv[2 * k + 1]
        tt(tmpb.ap, oh.ap, smat.ap, ALU.mult, [oh.res, smat.res], [tmpb.res])
        C.op("dve", lambda: nc.vector.tensor_reduce(out=s_t.ap, in_=v3(tmpb, 64), axis=AX.X, op=ALU.add), [tmpb.res], [s_t.res])
        tt(tmpb.ap, oh.ap, valid.ap, ALU.mult, [oh.res, valid.res], [tmpb.res])
        C.op("dve", lambda: nc.vector.tensor_reduce(out=v_t.ap, in_=v3(tmpb, 64), axis=AX.X, op=ALU.add), [tmpb.res], [v_t.res])
        sl = slf[k]
        ts(sl.ap, s_t.ap, dummyc.ap, None, ALU.subtract, None, [s_t.res, dummyc.res], [sl.res])
        tt(sl.ap, sl.ap, v_t.ap, ALU.mult, [sl.res, v_t.res], [sl.res])
        ts(sl.ap, sl.ap, dummyc.ap, None, ALU.add, None, [sl.res, dummyc.res], [sl.res])
        sl_t, g_t, w_t = (slot1_i, g1s, w1) if k == 0 else (slot2_i, g2s, w2)
        vcopy(sl_t.ap, sl.ap, [sl.res], [sl_t.res])
        tt(g_t.ap, gval.ap, w_t.ap, ALU.mult, [gval.res, w_t.res], [g_t.res])
        tt(g_t.ap, g_t.ap, v_t.ap, ALU.mult, [g_t.res, v_t.res], [g_t.res])
    if debug:
        st("sp", rt_d, Af.ap[:, 0:64], Af.res, [Af.res])

    pbarrier()
    sb.off = base_off
    x1f = [sb.alloc(2048, F32) for _ in range(2)]
    x1b = [sb.alloc(2048, BF16) for _ in range(2)]

    def s_loads(ti):
        ld("sp", x1f[ti % 2].ap, x1s_d[ti * 128:ti * 128 + 128, :], x1f[ti % 2].res, [x1f[ti % 2].res])

    s_loads(0)
    for tile_i in range(16):
        if tile_i + 1 < 16:
            s_loads(tile_i + 1)
        xf, xb = x1f[tile_i % 2], x1b[tile_i % 2]
        if tile_i % 2 == 0:
            act(xb.ap, xf.ap, AF.Copy, [xf.res], [xb.res])
        else:
            vcopy(xb.ap, xf.ap, [xf.res], [xb.res])
        for sl_t in (slot1_i, slot2_i):
            C.dma("pool", lambda sl_t=sl_t, xb=xb: nc.gpsimd.indirect_dma_start(
                out=xs_d, out_offset=bass.IndirectOffsetOnAxis(ap=sl_t.ap[:, tile_i:tile_i + 1], axis=0),
                in_=xb.ap, in_offset=None, bounds_check=BR, oob_is_err=False),
                xb.res, [xb.res, sl_t.res], ())

    C.barrier()
    sb.off = base_off

    wgs = [sb.alloc(16 * 512, BF16) for _ in range(2)]
    wus = [sb.alloc(16 * 512, BF16) for _ in range(2)]
    wds = [sb.alloc(4 * 2048, BF16) for _ in range(2)]
    xgs = [sb.alloc(2048, BF16) for _ in range(2)]
    xgTs = [sb.alloc(2048, BF16) for _ in range(2)]
    sg = sb.alloc(512, F32)
    hdn = [sb.alloc(512, BF16) for _ in range(2)]
    hdnT = [sb.alloc(512, BF16) for _ in range(2)]
    ysts = [sb.alloc(2048, F32) for _ in range(2)]
    ycnt = 0

    def d_loads(e):
        wg, wu, wd, xg = wgs[e % 2], wus[e % 2], wds[e % 2], xgs[e % 2]
        ld("sp", xg.ap, xs_d[e * 128:(e + 1) * 128, :], xg.res, [xg.res])
        if PRESTAGE and e < NPRE:
            ld("sp", wg.ap, wgb_d[e], wg.res, [wg.res])
            ld("sp", wu.ap, wub_d[e], wu.res, [wu.res])
            ld("sp", wd.ap, wdb_d[e], wd.res, [wd.res])
        else:
            C.dma("pool", lambda: nc.gpsimd.dma_start(out=wg.v3(16), in_=kp(w_gate[e])), wg.res, (), [wg.res])
            C.dma("pool", lambda: nc.gpsimd.dma_start(out=wu.v3(16), in_=kp(w_up[e])), wu.res, (), [wu.res])
            C.dma("pool", lambda: nc.gpsimd.dma_start(out=wd.v3(4), in_=kp(w_down[e])), wd.res, (), [wd.res])

    d_loads(0)
    for e in range(64):
        s = e % 2
        wg, wu, wd, xg, xgT, hd, hT, yst = wgs[s], wus[s], wds[s], xgs[s], xgTs[s], hdn[s], hdnT[s], ysts[s]
        if e + 1 < 64:
            d_loads(e + 1)
        wg3, wu3, wd3 = wg.v3(16), wu.v3(16), wd.v3(4)
        for kc in range(16):
            bb = kc // 8
            tr(psbf(bb, (kc % 8) * 128, (kc % 8 + 1) * 128), xg.ap[:, kc * 128:(kc + 1) * 128], ident_b.ap,
               [xg.res, ident_b.res], [pres[bb]])
        act(xgT.ap[:, 0:1024], psbf(0, 0, 1024), AF.Copy, [pres[0]], [xgT.res])
        vcopy(xgT.ap[:, 1024:2048], psbf(1, 0, 1024), [pres[1]], [xgT.res])
        for kc in range(16):
            mm(psb(2), xgT.ap[:, kc * 128:(kc + 1) * 128], wg3[:, kc, :], kc == 0, kc == 15, [xgT.res, wg.res], [pres[2]])
        for kc in range(16):
            mm(psb(3), xgT.ap[:, kc * 128:(kc + 1) * 128], wu3[:, kc, :], kc == 0, kc == 15, [xgT.res, wu.res], [pres[3]])
        act(sg.ap, psb(2), AF.Silu, [pres[2]], [sg.res])
        tt(hd.ap, psb(3), sg.ap, ALU.mult, [pres[3], sg.res], [hd.res])
        for fc in range(4):
            tr(psbf(4, fc * 128, (fc + 1) * 128), hd.ap[:, fc * 128:(fc + 1) * 128], ident_b.ap, [hd.res, ident_b.res], [pres[4]])
        vcopy(hT.ap, psbf(4, 0, 512), [pres[4]], [hT.res])
        for nb in range(4):
            yb = 5 + ycnt % 3
            ycnt += 1
            for fc in range(4):
                mm(psb(yb), hT.ap[:, fc * 128:(fc + 1) * 128], wd3[:, fc, nb * 512:(nb + 1) * 512], fc == 0, fc == 3,
                   [hT.res, wd.res], [pres[yb]])
            if nb % 2 == 0:
                act(yst.ap[:, nb * 512:(nb + 1) * 512], psb(yb), AF.Copy, [pres[yb]], [yst.res])
            else:
                vcopy(yst.ap[:, nb * 512:(nb + 1) * 512], psb(yb), [pres[yb]], [yst.res])
        st("sp", ys_d[e * 128:(e + 1) * 128, :], yst.ap, yst.res, [yst.res])

    C.barrier()
    sb.off = base_off

    wpg = sb.alloc(16 * 2048, BF16)
    wple = sb.alloc(2 * 2048, BF16)
    pTt = sb.alloc(2 * 2048, BF16)
    ln2g = sb.alloc(2048, F32)
    ln2b = sb.alloc(2048, F32)
    pleg = sb.alloc(2048, F32)
    yas = [sb.alloc(2048, F32) for _ in range(3)]
    ybs = [sb.alloc(2048, F32) for _ in range(3)]
    zs = [sb.alloc(2048, F32) for _ in range(3)]
    gates = ybs
    x2Ts = [sb.alloc(2048, BF16) for _ in range(2)]
    statss = [sb.alloc(24, F32) for _ in range(2)]
    mvs = [sb.alloc(2, F32) for _ in range(2)]
    sds = [sb.alloc(1, F32) for _ in range(2)]
    rstds = [sb.alloc(1, F32) for _ in range(2)]
    nmrs = [sb.alloc(1, F32) for _ in range(2)]
    sss = [sb.alloc(1, F32) for _ in range(2)]
    sd2s = [sb.alloc(1, F32) for _ in range(2)]
    rr2s = [sb.alloc(1, F32) for _ in range(2)]
    wpg3 = wpg.v3(16)
    for g in range(4):
        C.dma("pool", lambda g=g: nc.gpsimd.dma_start(out=wpg3[:, 4 * g:4 * g + 4, :], in_=kp(w_pg)[:, 4 * g:4 * g + 4, :]),
              wpg.res, (), [wpg.res])
    C.dma("pool", lambda: nc.gpsimd.dma_start(out=wple.v3(2), in_=kp(w_ple)), wple.res, (), [wple.res])
    C.dma("pool", lambda: nc.gpsimd.dma_start(out=pTt.v3(2), in_=kp(pT_d)), pTt.res, (), [pTt.res])
    ld("sp", ln2g.ap, ln2g_d, ln2g.res, [ln2g.res])
    ld("sp", ln2b.ap, ln2b_d, ln2b.res, [ln2b.res])
    ld("sp", pleg.ap, pleg_d, pleg.res, [pleg.res])
    wple3, pTt3 = wple.v3(2), pTt.v3(2)

    def e_loads(ti):
        k = ti % 3
        for (yt_, sl_t) in ((yas[k], slot1_i), (ybs[k], slot2_i)):
            C.dma("pool", lambda yt_=yt_, sl_t=sl_t: nc.gpsimd.indirect_dma_start(
                out=yt_.ap, out_offset=None, in_=ys_d,
                in_offset=bass.IndirectOffsetOnAxis(ap=sl_t.ap[:, ti:ti + 1], axis=0),
                bounds_check=BR, oob_is_err=False), yt_.res, [sl_t.res], [yt_.res])
        ld("sp", zs[k].ap, x1s_d[ti * 128:ti * 128 + 128, :], zs[k].res, [zs[k].res])

    def e_s1(tile_i):
        k = tile_i % 2
        k3 = tile_i % 3
        ya, yb_, z = yas[k3], ybs[k3], zs[k3]
        act(z.ap, z.ap, AF.Copy, [z.res], [z.res], scale=ALPHA)
        stt(z.ap, ya.ap, g1s.ap[:, tile_i:tile_i + 1], z.ap, ALU.mult, ALU.add, [ya.res, g1s.res, z.res], [z.res])
        stt(z.ap, yb_.ap, g2s.ap[:, tile_i:tile_i + 1], z.ap, ALU.mult, ALU.add, [yb_.res, g2s.res, z.res], [z.res])
        layer_norm(z, ln2g, ln2b, k)

    def e_s2a(tile_i):
        k = tile_i % 2
        k3 = tile_i % 3
        z, x2T = zs[k3], x2Ts[k]
        for kc in range(16):
            bb = 4 + kc // 4
            tr(psb(bb, (kc % 4) * 128, (kc % 4 + 1) * 128), z.ap[:, kc * 128:(kc + 1) * 128], ident_f.ap,
               [z.res, ident_f.res], [pres[bb]])
        for q in range(4):
            if q % 2 == 0:
                act(x2T.ap[:, q * 512:(q + 1) * 512], psb(4 + q), AF.Copy, [pres[4 + q]], [x2T.res])
            else:
                vcopy(x2T.ap[:, q * 512:(q + 1) * 512], psb(4 + q), [pres[4 + q]], [x2T.res])

    def e_s2b(tile_i):
        k = tile_i % 2
        k3 = tile_i % 3
        r0 = tile_i * 128
        ya, yb_, z, gate, x2T, ss, sd2, rr2 = yas[k3], ybs[k3], zs[k3], gates[k3], x2Ts[k], sss[k], sd2s[k], rr2s[k]
        for nb in range(4):
            for kc in range(16):
                mm(psb(nb), x2T.ap[:, kc * 128:(kc + 1) * 128], wpg3[:, kc, nb * 512:(nb + 1) * 512], kc == 0, kc == 15,
                   [x2T.res, wpg.res], [pres[nb]])
            act(gate.ap[:, nb * 512:(nb + 1) * 512], psb(nb), AF.Sigmoid, [pres[nb]], [gate.res])
        for nb in range(4):
            for qc in range(2):
                mm(psb(4 + nb), pTt3[:, qc, r0:r0 + 128], wple3[:, qc, nb * 512:(nb + 1) * 512], qc == 0, qc == 1,
                   [pTt.res, wple.res], [pres[4 + nb]])
            tt(gate.ap[:, nb * 512:(nb + 1) * 512], psb(4 + nb), gate.ap[:, nb * 512:(nb + 1) * 512], ALU.mult,
               [pres[4 + nb], gate.res], [gate.res])

    def e_s3(tile_i):
        k = tile_i % 2
        k3 = tile_i % 3
        r0 = tile_i * 128
        ya, yb_, z, gate, x2T, ss, sd2, rr2 = yas[k3], ybs[k3], zs[k3], gates[k3], x2Ts[k], sss[k], sd2s[k], rr2s[k]
        act(ya.ap, gate.ap, AF.Square, [gate.res], [ya.res, ss.res], accum_out=ss.ap)
        act(sd2.ap, ss.ap, AF.Sqrt, [ss.res, epsc.res], [sd2.res], scale=1.0 / D, bias=epsc.ap[:, 1:2])
        recip(rr2.ap, sd2.ap, [sd2.res], [rr2.res])
        stt(gate.ap, gate.ap, rr2.ap, pleg.ap, ALU.mult, ALU.mult, [gate.res, rr2.res, pleg.res], [gate.res])
        tt(gate.ap, gate.ap, z.ap, ALU.add, [gate.res, z.res], [gate.res])
        st("sp", out_d[r0:r0 + 128, :], gate.ap, gate.res, [gate.res])

    e_loads(0)
    e_loads(1)
    e_loads(2)
    e_s1(0)
    e_s2a(0)
    for tile_i in range(16):
        if tile_i + 1 < 16:
            e_s1(tile_i + 1)
        e_s2b(tile_i)
        if tile_i + 1 < 16:
            e_s2a(tile_i + 1)
        e_s3(tile_i)
        if tile_i + 3 < 16:
            e_loads(tile_i + 3)

    C.barrier()
    es.close()
    return nc, C


_CACHE = {}


def _consts():
    ident = np.eye(128, dtype=np.float32)
    tri = np.triu(np.ones((128, 128), np.float32), 1)
    inv_freq = (1.0 / (10000.0 ** (np.arange(0, 64, 2, dtype=np.float32) / 64.0))).astype(np.float32)
    ropec = np.zeros((64, 4), np.float32)
    ropec[:, 0] = np.concatenate([inv_freq, inv_freq])
    ropec[:32, 1] = -1.0
    ropec[32:, 1] = 1.0
    ebase = np.broadcast_to((np.arange(64, dtype=np.float32) * 128.0)[None, :], (128, 64)).copy()
    dummy = (NSLOT + np.arange(128, dtype=np.float32)).reshape(128, 1)
    return ident, tri, ropec, ebase, dummy


def _build_natab(rpb, half):
    tab = np.full((8, 5, 128, 7, 128), -30000.0, np.float32)
    own_row0 = 32 * half
    qc = np.arange(64)
    c0 = np.clip(qc - 8, 0, 48)
    for ty in range(5):
        jl = {0: 0, 1: 1, 2: 2, 3: 14, 4: 15}[ty]
        for qr in range(2):
            r = own_row0 + 2 * jl + qr
            start = min(max(r - 4, 0), 56)
            for ci in range(7):
                for kr in range(2):
                    krow = own_row0 + 2 * (jl + ci - 3) + kr
                    if krow < start or krow > start + 7 or krow < 0 or krow > 63:
                        continue
                    ro = krow - r + 7
                    for w in range(16):
                        kcs = c0 + w
                        vals = rpb[:, ro, kcs - qc + 15]
                        tab[:, ty, kr * 64 + kcs, ci, qr * 64 + qc] = vals
    return tab.reshape(8, 5, 128, 896)


def kernel(x, p, positions, w_in, rpb, q_norm_g, kv_norm_g, w_uq, w_uk, w_uv, w_o,
           ln1_g, ln1_b, w_group, b_group, w_router, b_router, w_gate, w_up, w_down,
           ln2_g, ln2_b, w_ple, w_ple_gate, ple_norm_g):
    f = lambda a: np.ascontiguousarray(np.asarray(a, dtype=np.float32))
    x = f(x); p = f(p); w_in0 = f(w_in)[0]
    positions = np.asarray(positions).astype(np.int32)
    debug = True
    if ("nc", debug) not in _CACHE:
        _CACHE[("nc", debug)] = build_program(debug)[0]
    nc = _CACHE[("nc", debug)]
    ident, tri, ropec, ebase, dummy = _consts()
    rep = lambda v: np.ascontiguousarray(np.broadcast_to(f(v).reshape(1, -1), (128, f(v).size)))
    s5 = 3 * 1024 + 1024
    w_krsw = np.ascontiguousarray(np.concatenate([w_in0[:, s5 + 32:s5 + 64], w_in0[:, s5:s5 + 32]], axis=1))
    wuq0 = f(w_uq)[0]
    wuq_h = wuq0.reshape(512, 8, 192)
    w_uqsw = np.ascontiguousarray(np.concatenate([wuq_h[:, :, 160:192], wuq_h[:, :, 128:160]], axis=2).reshape(512, 512))
    w_rt = np.ascontiguousarray(np.concatenate([f(w_group)[0], f(w_router)[0]], axis=1))
    b_rt = rep(np.concatenate([f(b_group)[0].reshape(-1), f(b_router)[0].reshape(-1)]))
    shared = {
        "w_in": w_in0, "w_krsw": w_krsw, "w_uq": wuq0, "w_uqsw": w_uqsw, "w_uk": f(w_uk)[0], "w_uv": f(w_uv)[0],
        "qg": np.ascontiguousarray(f(q_norm_g)[0].reshape(4, 128).T), "kvg": np.ascontiguousarray(f(kv_norm_g)[0].reshape(4, 128).T),
        "w_o": f(w_o)[0], "ln1g": rep(f(ln1_g)[0]), "ln1b": rep(f(ln1_b)[0]), "ln2g": rep(f(ln2_g)[0]), "ln2b": rep(f(ln2_b)[0]),
        "pleg": rep(f(ple_norm_g)[0]), "w_rt": w_rt, "b_rt": b_rt, "w_gate": f(w_gate)[0], "w_up": f(w_up)[0],
        "w_down": f(w_down)[0], "w_ple": f(w_ple)[0], "w_pg": f(w_ple_gate)[0],
        "ident": ident, "tri": tri, "ropec": ropec, "ebase": ebase, "dummyc": dummy,
    }
    rpb0 = f(rpb)[0]
    natabs = [_build_natab(rpb0, 0), _build_natab(rpb0, 1)]
    in_maps = []
    for c in range(8):
        b, half = c // 2, c % 2
        own = x[b, half * 2048:(half + 1) * 2048]
        oth = x[b, (1 - half) * 2048:(2 - half) * 2048]
        halo = np.zeros((768, D), np.float32)
        if half == 0:
            halo[384:768] = oth[0:384]
        else:
            halo[0:384] = oth[2048 - 384:2048]
        pos = np.concatenate([positions[b, half * 2048:(half + 1) * 2048], positions[b, (1 - half) * 2048:(2 - half) * 2048]])
        m = dict(shared)
        m.update({
            "xT_own": np.ascontiguousarray(own.T), "xT_oth": np.ascontiguousarray(oth.T),
            "xT_halo": np.ascontiguousarray(halo.T), "x_own": np.ascontiguousarray(own),
            "pT": np.ascontiguousarray(p[0, b, half * 2048:(half + 1) * 2048].T),
            "posb": np.ascontiguousarray(np.broadcast_to(pos[None, :], (64, 4096))).astype(np.int32),
            "natab": natabs[half],
        })
        in_maps.append(m)
    res = run_bass_kernel_spmd(nc, in_maps, core_ids=list(range(8)))
    out = np.empty((4, 4096, D), np.float32)
    for c in range(8):
        b, half = c // 2, c % 2
        out[b, half * 2048:(half + 1) * 2048] = res.results[c]["out"]
    if debug:
        _CACHE["dbg"] = res.results
    return out
```
